# Optimizing a Trainium2 kernel written in Bass

```python
import math
import jax, jax.numpy as jnp
from jax import lax
import numpy as np

D_MODEL = 4096
BATCH = 4
SEQ = 4096
DEPTH = 1

HEAD_DIM = 128
ATTN_GROUPS = ((128, 1), (512, 4), (2048, 16))
HEADS_PER_GROUP = 8
N_ATTN_HEADS = HEADS_PER_GROUP * len(ATTN_GROUPS)
ATTN_WIDTH = N_ATTN_HEADS * HEAD_DIM
ATTN_OUT_WIDTH = HEADS_PER_GROUP * HEAD_DIM
ROT_DIM = HEAD_DIM // 4
ROPE_THETA = 500000.0
ATTN_BLOCK = 128
LRU_WIDTH = 2048
LRU_BLOCKS = 8
LRU_BLOCK_W = LRU_WIDTH // LRU_BLOCKS
CONV_WIDTH = 4
RG_C = 8.0
N_GROUPS = 8
EXPERTS_PER_GROUP = 8
N_EXPERTS = N_GROUPS * EXPERTS_PER_GROUP
TOP_K = 2
D_EXPERT = 512
MOE_BLOCK = 128
LN_EPS = 1e-5
DEEPNORM_ALPHA = (2 * DEPTH) ** 0.25
DEEPNORM_BETA = (8 * DEPTH) ** -0.25
Q_OFF = 0
K_OFF = ATTN_WIDTH
V_OFF = 2 * ATTN_WIDTH
RX_OFF = 3 * ATTN_WIDTH
RG_OFF = RX_OFF + LRU_WIDTH
GATE_OFF = RG_OFF + LRU_WIDTH
IN_COLS = GATE_OFF + 2 * D_MODEL

kernel_name = 'hybrid_dilated_attn_rglru_hmoe_block'


def layer_norm(t, g, b):
    tf = t.astype(jnp.float32)
    mu = jnp.mean(tf, axis=-1, keepdims=True)
    var = jnp.mean(jnp.square(tf - mu), axis=-1, keepdims=True)
    y = (tf - mu) * lax.rsqrt(var + LN_EPS) * g.astype(jnp.float32) + b.astype(jnp.float32)
    return y.astype(t.dtype)


def partial_rope(t, positions):
    half = ROT_DIM // 2
    inv_freq = jnp.power(jnp.float32(ROPE_THETA), -jnp.arange(half, dtype=jnp.float32) * 2.0 / ROT_DIM)
    ang = positions.astype(jnp.float32)[..., None] * inv_freq
    cos = jnp.cos(ang)[:, :, None, :]
    sin = jnp.sin(ang)[:, :, None, :]
    tf = t.astype(jnp.float32)
    t1, t2, rest = tf[..., :half], tf[..., half:ROT_DIM], tf[..., ROT_DIM:]
    out = jnp.concatenate([t1 * cos - t2 * sin, t2 * cos + t1 * sin, rest], axis=-1)
    return out.astype(t.dtype)


def dilated_window_attention(q, k, v, window, dilation):
    B, S, Hg, hd = q.shape
    L = S // dilation
    span = window // dilation
    nb = -(-L // ATTN_BLOCK)
    Lp = nb * ATTN_BLOCK

    def to_blocks(t):
        t = t.reshape(B, L, dilation, Hg, hd).transpose(0, 2, 3, 1, 4)
        t = jnp.pad(t, ((0, 0), (0, 0), (0, 0), (0, Lp - L), (0, 0)))
        return t.reshape(B, dilation, Hg, nb, ATTN_BLOCK, hd)

    def with_prev(t):
        prev = jnp.pad(t, ((0, 0), (0, 0), (0, 0), (1, 0), (0, 0), (0, 0)))[:, :, :, :nb]
        return jnp.concatenate([prev, t], axis=4)

    qb = to_blocks(q)
    kk = with_prev(to_blocks(k))
    vv = with_prev(to_blocks(v))
    s = jnp.einsum('brhnqc,brhnkc->brhnqk', qb, kk).astype(jnp.float32)
    blk = jnp.arange(nb)[:, None] * ATTN_BLOCK
    qpos = blk + jnp.arange(ATTN_BLOCK)[None, :]
    kpos = blk - ATTN_BLOCK + jnp.arange(2 * ATTN_BLOCK)[None, :]
    rel = qpos[:, :, None] - kpos[:, None, :]
    valid = (rel >= 0) & (rel <= span) & (kpos[:, None, :] >= 0)
    s = jnp.where(valid, s, -jnp.inf)
    lse = jax.nn.logsumexp(s, axis=-1)
    p = jnp.exp(s - lse[..., None])
    o = jnp.einsum('brhnqk,brhnkc->brhnqc', p.astype(v.dtype), vv)
    o = o.reshape(B, dilation, Hg, Lp, hd)[:, :, :, :L].transpose(0, 3, 1, 2, 4).reshape(B, S, Hg, hd)
    lse = lse.reshape(B, dilation, Hg, Lp)[:, :, :, :L].transpose(0, 3, 1, 2).reshape(B, S, Hg)
    return o, lse


def causal_depthwise_conv(t, conv_w, conv_b):
    W = t.shape[-1]
    y = lax.conv_general_dilated(t, conv_w.reshape(CONV_WIDTH, 1, W).astype(t.dtype), window_strides=(1,),
                                 padding=[(CONV_WIDTH - 1, 0)], dimension_numbers=('NWC', 'WIO', 'NWC'),
                                 feature_group_count=W)
    return y + conv_b


def rg_lru(xr, w_a, b_a, w_x, b_x, lam):
    B, S, W = xr.shape
    xf = xr.astype(jnp.float32)
    xb = xf.reshape(B, S, LRU_BLOCKS, LRU_BLOCK_W)
    r = jax.nn.sigmoid(jnp.einsum('bsni,nij->bsnj', xb, w_a.astype(jnp.float32)) + b_a.astype(jnp.float32)).reshape(B, S, W)
    i = jax.nn.sigmoid(jnp.einsum('bsni,nij->bsnj', xb, w_x.astype(jnp.float32)) + b_x.astype(jnp.float32)).reshape(B, S, W)
    log_a = -RG_C * r * jax.nn.softplus(-lam.astype(jnp.float32))
    a = jnp.exp(log_a)
    u = jnp.sqrt(-jnp.expm1(2.0 * log_a)) * (i * xf)

    def combine(left, right):
        al, bl = left
        ar, br = right
        return ar * al, ar * bl + br

    _, h = lax.associative_scan(combine, (a, u), axis=1)
    return h


def token_mixer(x, positions, w_in, b_gate, conv_w, conv_b, w_rg_a, b_rg_a, w_rg_x, b_rg_x,
                lru_lambda, w_attn_proj, w_rec_proj, w_out):
    B, S, _ = x.shape
    proj = jnp.einsum('bsd,dc->bsc', x, w_in)
    q = proj[..., Q_OFF:K_OFF].reshape(B, S, N_ATTN_HEADS, HEAD_DIM)
    k = proj[..., K_OFF:V_OFF].reshape(B, S, N_ATTN_HEADS, HEAD_DIM)
    v = proj[..., V_OFF:RX_OFF].reshape(B, S, N_ATTN_HEADS, HEAD_DIM)
    q = partial_rope(q, positions) * (HEAD_DIM ** -0.5)
    k = partial_rope(k, positions)
    outs, lses = [], []
    for g, (window, dilation) in enumerate(ATTN_GROUPS):
        hs = slice(g * HEADS_PER_GROUP, (g + 1) * HEADS_PER_GROUP)
        o, l = dilated_window_attention(q[:, :, hs], k[:, :, hs], v[:, :, hs], window, dilation)
        outs.append(o)
        lses.append(l)
    mix_w = jax.nn.softmax(jnp.stack(lses, axis=0), axis=0)
    attn = jnp.einsum('gbsh,gbshc->bshc', mix_w, jnp.stack(outs, axis=0).astype(jnp.float32))
    attn = attn.reshape(B, S, ATTN_OUT_WIDTH).astype(x.dtype)
    xr = causal_depthwise_conv(proj[..., RX_OFF:RG_OFF], conv_w, conv_b)
    gate_r = jax.nn.gelu(proj[..., RG_OFF:GATE_OFF].astype(jnp.float32))
    h = rg_lru(xr, w_rg_a, b_rg_a, w_rg_x, b_rg_x, lru_lambda)
    rec = (h * gate_r).astype(x.dtype)
    gates = jax.nn.sigmoid((proj[..., GATE_OFF:].reshape(B, S, 2, D_MODEL) + b_gate).astype(jnp.float32))
    y_a = jnp.einsum('bsc,cd->bsd', attn, w_attn_proj).astype(jnp.float32)
    y_r = jnp.einsum('bsc,cd->bsd', rec, w_rec_proj).astype(jnp.float32)
    merged = (gates[:, :, 0] * y_a + gates[:, :, 1] * y_r).astype(x.dtype)
    return jnp.einsum('bsd,de->bse', merged, w_out)


def hierarchical_moe(xt, w_router_group, b_router_group, w_router_expert, b_router_expert, w_gate, w_up, w_down):
    T, D = xt.shape
    logits_g = (xt @ w_router_group + b_router_group).astype(jnp.float32)
    p_g = jax.nn.softmax(logits_g, axis=-1)
    g_idx = jnp.argmax(logits_g, axis=-1)
    p_grp = jnp.take_along_axis(p_g, g_idx[:, None], axis=-1)
    logits_e = (xt @ w_router_expert + b_router_expert).astype(jnp.float32).reshape(T, N_GROUPS, EXPERTS_PER_GROUP)
    le = jnp.take_along_axis(logits_e, g_idx[:, None, None], axis=1)[:, 0]
    top_v, top_i = lax.top_k(le, TOP_K)
    weight = (p_grp * jax.nn.softmax(top_v, axis=-1)).astype(xt.dtype)
    expert_id = g_idx[:, None].astype(jnp.int32) * EXPERTS_PER_GROUP + top_i.astype(jnp.int32)

    A = T * TOP_K
    flat_e = expert_id.reshape(-1)
    flat_tok = jnp.broadcast_to(jnp.arange(T, dtype=jnp.int32)[:, None], (T, TOP_K)).reshape(-1)
    flat_w = weight.reshape(-1)
    order = jnp.argsort(flat_e)
    se = flat_e[order]
    counts = jnp.bincount(flat_e, length=N_EXPERTS)
    starts = jnp.cumsum(counts) - counts
    padded = (counts + MOE_BLOCK - 1) // MOE_BLOCK * MOE_BLOCK
    pends = jnp.cumsum(padded)
    pstarts = pends - padded
    dest = pstarts[se] + jnp.arange(A, dtype=jnp.int32) - starts[se]
    n_blocks = (A + MOE_BLOCK - 1) // MOE_BLOCK + N_EXPERTS
    rows = n_blocks * MOE_BLOCK
    row_tok = jnp.full((rows,), T, dtype=jnp.int32).at[dest].set(flat_tok[order])
    row_w = jnp.zeros((rows,), xt.dtype).at[dest].set(flat_w[order])
    block_e = jnp.minimum(jnp.searchsorted(pends, jnp.arange(n_blocks) * MOE_BLOCK, side='right'), N_EXPERTS - 1)
    xpad = jnp.concatenate([xt, jnp.zeros((1, D), xt.dtype)], axis=0)

    def run_block(args):
        tok, w, e = args
        xb = xpad[tok]
        hid = jax.nn.silu(xb @ w_gate[e]) * (xb @ w_up[e])
        return (hid @ w_down[e]) * w[:, None]

    outs = lax.map(run_block, (row_tok.reshape(n_blocks, MOE_BLOCK), row_w.reshape(n_blocks, MOE_BLOCK), block_e))
    y = jnp.zeros((T + 1, D), outs.dtype).at[row_tok].add(outs.reshape(rows, D))
    return y[:T]


def setup_inputs(seed: int = 0) -> dict:
    key = jax.random.key(seed)
    ks = jax.random.split(key, 26)
    L_ = DEPTH
    f32 = jnp.float32
    nrm = lambda k, shape, scale: jax.random.normal(k, shape, f32) * scale
    x = jax.random.normal(ks[0], (BATCH, SEQ, D_MODEL), f32)
    positions = (jax.random.randint(ks[1], (BATCH, 1), 0, 1024) + jnp.arange(SEQ)[None, :]).astype(jnp.int32)
    w_in = nrm(ks[2], (L_, D_MODEL, IN_COLS), D_MODEL ** -0.5)
    w_in = w_in.at[:, :, V_OFF:RX_OFF].multiply(DEEPNORM_BETA)
    b_gate = nrm(ks[3], (L_, 2, D_MODEL), 0.02)
    conv_w = nrm(ks[4], (L_, CONV_WIDTH, LRU_WIDTH), CONV_WIDTH ** -0.5)
    conv_b = nrm(ks[5], (L_, LRU_WIDTH), 0.02)
    w_rg_a = nrm(ks[6], (L_, LRU_BLOCKS, LRU_BLOCK_W, LRU_BLOCK_W), LRU_BLOCK_W ** -0.5)
    b_rg_a = nrm(ks[7], (L_, LRU_BLOCKS, LRU_BLOCK_W), 0.02)
    w_rg_x = nrm(ks[8], (L_, LRU_BLOCKS, LRU_BLOCK_W, LRU_BLOCK_W), LRU_BLOCK_W ** -0.5)
    b_rg_x = nrm(ks[9], (L_, LRU_BLOCKS, LRU_BLOCK_W), 0.02)
    a_c = jax.random.uniform(ks[10], (L_, LRU_WIDTH), f32, 0.9, 0.999)
    a0 = a_c ** (1.0 / RG_C)
    lru_lambda = jnp.log(a0) - jnp.log1p(-a0)
    w_attn_proj = nrm(ks[11], (L_, ATTN_OUT_WIDTH, D_MODEL), ATTN_OUT_WIDTH ** -0.5)
    w_rec_proj = nrm(ks[12], (L_, LRU_WIDTH, D_MODEL), LRU_WIDTH ** -0.5)
    w_out = nrm(ks[13], (L_, D_MODEL, D_MODEL), D_MODEL ** -0.5 * DEEPNORM_BETA)
    ln1_g = 1.0 + nrm(ks[14], (L_, D_MODEL), 0.02)
    ln1_b = nrm(ks[15], (L_, D_MODEL), 0.02)
    w_router_group = nrm(ks[16], (L_, D_MODEL, N_GROUPS), D_MODEL ** -0.5)
    b_router_group = nrm(ks[17], (L_, N_GROUPS), 0.01)
    w_router_expert = nrm(ks[18], (L_, D_MODEL, N_EXPERTS), D_MODEL ** -0.5)
    b_router_expert = nrm(ks[19], (L_, N_EXPERTS), 0.01)
    w_gate = nrm(ks[20], (L_, N_EXPERTS, D_MODEL, D_EXPERT), D_MODEL ** -0.5)
    w_up = nrm(ks[21], (L_, N_EXPERTS, D_MODEL, D_EXPERT), D_MODEL ** -0.5)
    w_down = nrm(ks[22], (L_, N_EXPERTS, D_EXPERT, D_MODEL), D_EXPERT ** -0.5 * DEEPNORM_BETA)
    ln2_g = 1.0 + nrm(ks[23], (L_, D_MODEL), 0.02)
    ln2_b = nrm(ks[24], (L_, D_MODEL), 0.02)
    return {'x': x, 'positions': positions, 'w_in': w_in, 'b_gate': b_gate, 'conv_w': conv_w, 'conv_b': conv_b,
            'w_rg_a': w_rg_a, 'b_rg_a': b_rg_a, 'w_rg_x': w_rg_x, 'b_rg_x': b_rg_x, 'lru_lambda': lru_lambda,
            'w_attn_proj': w_attn_proj, 'w_rec_proj': w_rec_proj, 'w_out': w_out, 'ln1_g': ln1_g, 'ln1_b': ln1_b,
            'w_router_group': w_router_group, 'b_router_group': b_router_group,
            'w_router_expert': w_router_expert, 'b_router_expert': b_router_expert,
            'w_gate': w_gate, 'w_up': w_up, 'w_down': w_down, 'ln2_g': ln2_g, 'ln2_b': ln2_b}


def reference(x, positions, w_in, b_gate, conv_w, conv_b, w_rg_a, b_rg_a, w_rg_x, b_rg_x, lru_lambda,
              w_attn_proj, w_rec_proj, w_out, ln1_g, ln1_b, w_router_group, b_router_group,
              w_router_expert, b_router_expert, w_gate, w_up, w_down, ln2_g, ln2_b):
    h = x
    B, S, D = x.shape
    for layer in range(DEPTH):
        mix = token_mixer(h, positions, w_in[layer], b_gate[layer], conv_w[layer], conv_b[layer],
                          w_rg_a[layer], b_rg_a[layer], w_rg_x[layer], b_rg_x[layer], lru_lambda[layer],
                          w_attn_proj[layer], w_rec_proj[layer], w_out[layer])
        h = layer_norm(DEEPNORM_ALPHA * h + mix, ln1_g[layer], ln1_b[layer])
        ff = hierarchical_moe(h.reshape(B * S, D), w_router_group[layer], b_router_group[layer],
                              w_router_expert[layer], b_router_expert[layer],
                              w_gate[layer], w_up[layer], w_down[layer]).reshape(B, S, D)
        h = layer_norm(DEEPNORM_ALPHA * h + ff, ln2_g[layer], ln2_b[layer])
    return h
```

```python
import math
from contextlib import ExitStack
import numpy as np
import concourse.bass as bass
import concourse.mybir as mybir
from concourse.bass_utils import run_bass_kernel_spmd

F32 = mybir.dt.float32
BF16 = mybir.dt.bfloat16
I32 = mybir.dt.int32
AF = mybir.ActivationFunctionType
ALU = mybir.AluOpType
AX = mybir.AxisListType

SAME_ENGINE_SYNC = True
N_DMA_SEMS = 24

S_LOC = 4096
OWN = 2048
D = 4096
NCH_IN = 168
ALPHA = 2.0 ** 0.25
EPS = 1e-5
MAGIC = 12582912.0
TWO_PI = 2.0 * math.pi


class Buf:
    __slots__ = ("name", "w", "r", "multi", "ws")

    def __init__(self, name="", multi=False):
        self.name = name
        self.w = None
        self.r = {}
        self.multi = multi
        self.ws = []


class Op:
    __slots__ = ("eng", "fn", "deps", "sig", "sigval", "is_dma", "dsem", "dval")

    def __init__(self, eng, fn, is_dma):
        self.eng = eng
        self.fn = fn
        self.deps = []
        self.sig = False
        self.sigval = None
        self.is_dma = is_dma
        self.dsem = None
        self.dval = None


class Eng:
    def __init__(self, fw, name):
        self.fw = fw
        self.name = name
        self.ops = []
        self.sem = None
        self.dsems = []
        self.pending = []

    def op(self, fn, reads=(), writes=(), is_dma=False):
        o = Op(self, fn, is_dma)
        deps = list(self.pending)
        self.pending = []
        for b in reads:
            if b.multi:
                deps.extend(b.ws)
            elif b.w is not None:
                deps.append(b.w)
        for b in writes:
            if b.multi:
                continue
            if b.w is not None:
                deps.append(b.w)
            deps.extend(b.r.values())
        seen = set()
        for d in deps:
            if d is o or id(d) in seen:
                continue
            seen.add(id(d))
            if (not d.is_dma) and d.eng is self and not is_dma:
                if self.name == "pe" or not SAME_ENGINE_SYNC:
                    continue
            d.sig = True
            o.deps.append(d)
        key = (self.name, is_dma)
        for b in reads:
            if b.multi:
                continue
            if is_dma:
                b.r[(key, len(b.r))] = o
            else:
                b.r[key] = o
        for b in writes:
            if b.multi:
                b.ws.append(o)
                continue
            b.w = o
            b.r = {}
        self.ops.append(o)
        self.fw.all_ops.append(o)
        return o

    def dma(self, out, in_, reads=(), writes=(), **kw):
        return self.op(lambda e: e.dma_start(out=out, in_=in_, **kw), reads, writes, is_dma=True)


class FW:
    def __init__(self, nc):
        self.nc = nc
        self.all_ops = []
        self.pe = Eng(self, "pe")
        self.act = Eng(self, "act")
        self.dve = Eng(self, "dve")
        self.pool = Eng(self, "pool")
        self.sp = Eng(self, "sp")
        self.engs = [self.pe, self.act, self.dve, self.pool, self.sp]
        self.mark = 0

    def barrier(self):
        lasts = []
        for e in self.engs:
            last_c = None
            for o in reversed(e.ops):
                if not o.is_dma:
                    last_c = o
                    break
            if last_c is not None:
                lasts.append(last_c)
        dm = []
        for e in self.engs:
            dl = [o for o in e.ops if o.is_dma]
            dm.extend(dl[-N_DMA_SEMS:])
        for e in self.engs:
            e.pending = list(e.pending) + lasts + dm

    def emit(self, stack):
        nc = self.nc
        for e in self.engs:
            e.sem = stack.enter_context(nc.semaphore("s_" + e.name))
            if e.name in ("sp", "act", "pool"):
                e.dsems = [stack.enter_context(nc.semaphore("d_%s_%d" % (e.name, i))) for i in range(N_DMA_SEMS)]
        for e in self.engs:
            c = 0
            k = 0
            uses = [0] * max(1, len(e.dsems))
            for o in e.ops:
                if o.is_dma:
                    s = k % len(e.dsems)
                    k += 1
                    uses[s] += 1
                    o.dsem = e.dsems[s]
                    o.dval = 16 * uses[s]
                elif o.sig:
                    c += 1
                    o.sigval = c
        block = stack.enter_context(nc.Block())

        def run(eng_obj, h):
            seen = {}

            def wait(sem, val):
                key = id(sem)
                if seen.get(key, 0) >= val:
                    return
                seen[key] = val
                h.wait_ge(sem, val)

            for o in eng_obj.ops:
                for d in o.deps:
                    if d.is_dma:
                        wait(d.dsem, d.dval)
                    else:
                        wait(d.eng.sem, d.sigval)
                if o.is_dma:
                    if o.dval > 16:
                        wait(o.dsem, o.dval - 16)
                    o.fn(h).then_inc(o.dsem, 16)
                else:
                    ins = o.fn(h)
                    if o.sig:
                        ins.then_inc(eng_obj.sem, 1)
            last = {}
            for o in eng_obj.ops:
                if o.is_dma:
                    last[id(o.dsem)] = (o.dsem, o.dval)
            for sem, val in last.values():
                wait(sem, val)

        @block.tensor
        def _(h):
            run(self.pe, h)

        @block.scalar
        def _(h):
            run(self.act, h)

        @block.vector
        def _(h):
            run(self.dve, h)

        @block.gpsimd
        def _(h):
            run(self.pool, h)

        @block.sync
        def _(h):
            run(self.sp, h)


class T:
    def __init__(self, h, name):
        self.h = h
        self.b = Buf(name)

    def __getitem__(self, k):
        return self.h[k]


def build_nc():
    nc = bass.Bass("TRN2", target_bir_lowering=False)

    def din(name, shape, dt=F32):
        return nc.dram_tensor(name, list(shape), dt, kind="ExternalInput").ap()

    def dscr(name, shape, dt):
        return nc.dram_tensor(name, list(shape), dt, kind="Internal").ap()

    xT = din("xT", [8, 128, 32 * 512])
    xown = din("xown", [OWN, D])
    pos = din("pos", [1, S_LOC], I32)
    win = din("win", [NCH_IN, 128, 4096])
    bgate = din("bgate", [128, 64])
    convw = din("convw", [128, 16 * 4])
    convb = din("convb", [128, 16])
    wrga = din("wrga", [8, 128, 512])
    wrgx = din("wrgx", [8, 128, 512])
    brga = din("brga", [128, 16])
    brgx = din("brgx", [128, 16])
    lam = din("lam", [128, 16])
    wpr = din("wpr", [32, 128, 3072])
    wout = din("wout", [32, 128, 4096])
    ln1g = din("ln1g", [1, D])
    ln1b = din("ln1b", [1, D])
    wrt = din("wrt", [128, 32 * 72])
    brt = din("brt", [1, 72])
    wg = din("wg", [64, 4, 128, 4096])
    wu = din("wu", [64, 4, 128, 4096])
    wd = din("wd", [64, 512, D])
    ln2g = din("ln2g", [1, D])
    ln2b = din("ln2b", [1, D])
    c_ident = din("c_ident", [128, 128])
    c_maskR = din("c_maskR", [128, 256])
    c_maskP = din("c_maskP", [128, 256])
    c_pm = din("c_pm", [32, 32])
    c_invf = din("c_invf", [32, 1])
    c_flag = din("c_flag", [128, 1])
    c_ustr = din("c_ustr", [128, 128])
    c_e128 = din("c_e128", [128, 64])
    c_dump = din("c_dump", [128, 1])
    out = nc.dram_tensor("out", [OWN, D], F32, kind="ExternalOutput").ap()

    QK = dscr("QK", [48, 128, S_LOC], BF16)
    VT = dscr("VT", [S_LOC, 3072], BF16)
    PT = dscr("PT", [96, 128, S_LOC], BF16)
    AT = dscr("AT", [8, 128, OWN], BF16)
    RT = dscr("RT", [16, 128, OWN], BF16)
    H1 = dscr("H1", [OWN, D], F32)
    WPB = dscr("WPB", [32, 128, 3072], BF16)
    WOB = dscr("WOB", [32, 128, 4096], BF16)
    XS = dscr("XS", [65 * 128, D], BF16)
    YS = [dscr("YS%d" % i, [65 * 128, 2048], F32) for i in range(2)]

    fw = FW(nc)
    pe, act, dve, pool, sp = fw.pe, fw.act, fw.dve, fw.pool, fw.sp
    b_QK, b_VT, b_PT, b_AT, b_RT, b_H1, b_XS, b_YS, b_out = [Buf(n, multi=True) for n in "QK VT PT AT RT H1 XS YS out".split()]
    b_XZ = Buf("XSzero", multi=True)

    with ExitStack() as top:
        def sbuf(st, name, shape, dt):
            return T(st.enter_context(nc.sbuf_tensor(name, list(shape), dt)), name)

        banks = [T(top.enter_context(nc.psum_tensor("bank%d" % i, [128, 512], F32)), "bank%d" % i) for i in range(8)]

        ident_f = sbuf(top, "ident_f", [128, 128], F32)
        ident_b = sbuf(top, "ident_b", [128, 128], BF16)
        ones_b = sbuf(top, "ones_b", [128, 128], BF16)
        maskR = sbuf(top, "maskR", [128, 256], BF16)
        maskP = sbuf(top, "maskP", [128, 256], BF16)
        pm = sbuf(top, "pm", [32, 32], BF16)
        invf = sbuf(top, "invf", [32, 1], F32)
        flag = sbuf(top, "flag", [128, 1], F32)
        ustr = sbuf(top, "ustr", [128, 128], BF16)
        e128 = sbuf(top, "e128", [128, 64], F32)
        dumpc = sbuf(top, "dumpc", [128, 1], F32)
        bg = sbuf(top, "bg", [128, 64], F32)
        destI = sbuf(top, "destI", [128, 32], I32)
        wts = sbuf(top, "wts", [128, 32], F32)

        sp.dma(ident_f[:], c_ident, writes=[ident_f.b])
        pool.dma(ident_b[:], c_ident, writes=[ident_b.b])
        pool.dma(maskR[:], c_maskR, writes=[maskR.b])
        pool.dma(maskP[:], c_maskP, writes=[maskP.b])
        pool.dma(pm[:], c_pm, writes=[pm.b])
        pool.dma(ustr[:], c_ustr, writes=[ustr.b])
        sp.dma(invf[:], c_invf, writes=[invf.b])
        sp.dma(flag[:], c_flag, writes=[flag.b])
        sp.dma(e128[:], c_e128, writes=[e128.b])
        sp.dma(dumpc[:], c_dump, writes=[dumpc.b])
        sp.dma(bg[:], bgate, writes=[bg.b])
        dve.op(lambda e: e.memset(ones_b[:], 1.0), writes=[ones_b.b])

        class Ring:
            def __init__(self, st, n, name):
                self.slots = [sbuf(st, "%s%d" % (name, i), [128, 4096], BF16) for i in range(n)]
                self.i = 0

            def load(self, src, nel=4096, k=None):
                s = self.slots[self.i % len(self.slots)]
                self.i += 1
                o = s[:, 0:nel]
                if k is not None:
                    o = o.rearrange("p (k e) -> p k e", k=k)
                pool.dma(o, src, writes=[s.b])
                return s

        with ExitStack() as st:
            ring = Ring(st, 8, "r1_")
            xt = [sbuf(st, "xt%d" % i, [128, 32, 512], BF16) for i in range(2)]
            stg = [sbuf(st, "stg%d" % i, [128, 512], BF16) for i in range(4)]
            posi = sbuf(st, "posi", [32, 512], I32)
            ang = sbuf(st, "ang", [32, 512], F32)
            tk = sbuf(st, "tk", [32, 512], F32)
            cos_t = sbuf(st, "cos_t", [32, 512], F32)
            sin_t = sbuf(st, "sin_t", [32, 512], F32)
            r1 = sbuf(st, "rp1", [32, 512], F32)
            r2 = sbuf(st, "rp2", [32, 512], F32)

            def load_xt(lt):
                t = xt[lt % 2]
                for q in range(4):
                    pool.dma(t[:, q * 8:(q + 1) * 8, :],
                             xT[lt][:, q * 4096:(q + 1) * 4096].rearrange("p (k t) -> p k t", k=8),
                             writes=[t.b])

            def trig(dst, shift):
                dve.op(lambda e: e.tensor_scalar(out=r1[:], in0=ang[:], scalar1=shift, scalar2=None, op0=ALU.add),
                       reads=[ang.b], writes=[r1.b])
                dve.op(lambda e: e.tensor_scalar(out=tk[:], in0=r1[:], scalar1=1.0 / TWO_PI, scalar2=MAGIC,
                                                 op0=ALU.mult, op1=ALU.add), reads=[r1.b], writes=[tk.b])
                dve.op(lambda e: e.tensor_scalar(out=tk[:], in0=tk[:], scalar1=-MAGIC, scalar2=None, op0=ALU.add),
                       reads=[tk.b], writes=[tk.b])
                dve.op(lambda e: e.scalar_tensor_tensor(out=r1[:], in0=tk[:], scalar=-TWO_PI, in1=r1[:],
                                                        op0=ALU.mult, op1=ALU.add), reads=[tk.b, r1.b], writes=[r1.b])
                dve.op(lambda e: e.tensor_scalar(out=r1[:], in0=r1[:], scalar1=3.14159, scalar2=-3.14159,
                                                 op0=ALU.min, op1=ALU.max), reads=[r1.b], writes=[r1.b])
                act.op(lambda e: e.activation(out=dst[:], in_=r1[:], func=AF.Sin), reads=[r1.b], writes=[dst.b])

            def chunks_for(lt):
                L = []
                own = lt >= 4
                for hd in range(24):
                    g = hd // 8
                    need_kv = own or g == 2 or lt == 3
                    if own:
                        L.append((hd, "q", hd))
                    if need_kv:
                        L.append((24 + hd, "k", 24 + hd))
                        L.append((48 + hd, "v", hd))
                for c in range(16):
                    L.append((72 + c, "p", c))
                if own:
                    for c in range(16):
                        L.append((88 + c, "p", 16 + c))
                    for c in range(64):
                        L.append((104 + c, "g", 32 + c))
                return L

            load_xt(0)
            cnt = 0
            for lt in range(8):
                x_t = xt[lt % 2]
                tok0 = lt * 512
                if lt + 1 < 8:
                    load_xt(lt + 1)
                sp.dma(posi[:], pos[0:1, tok0:tok0 + 512].partition_broadcast(32), writes=[posi.b])
                dve.op(lambda e: e.tensor_copy(out=ang[:], in_=posi[:]), reads=[posi.b], writes=[ang.b])
                dve.op(lambda e: e.tensor_scalar(out=ang[:], in0=ang[:], scalar1=invf[:, 0:1], scalar2=None, op0=ALU.mult),
                       reads=[ang.b, invf.b], writes=[ang.b])
                trig(sin_t, 0.0)
                trig(cos_t, math.pi / 2)
                for (wc, kind, di) in chunks_for(lt):
                    wt = ring.load(win[wc])
                    wv = wt[:, :].rearrange("p (k c) -> p k c", k=32)
                    bk = banks[cnt % 4]
                    sg = stg[cnt % 4]
                    cnt += 1
                    if kind == "v":
                        for s in range(4):
                            for k in range(32):
                                pe.op(lambda e, s=s, k=k, bk=bk, wv=wv, x_t=x_t: e.matmul(
                                    bk[:, s * 128:(s + 1) * 128], x_t[:, k, s * 128:(s + 1) * 128], wv[:, k, :],
                                    start=(k == 0), stop=(k == 31)),
                                    reads=[x_t.b, wt.b], writes=[bk.b])
                        act.op(lambda e, bk=bk, sg=sg: e.activation(out=sg[:], in_=bk[:], func=AF.Copy),
                               reads=[bk.b], writes=[sg.b])
                        sp.dma(VT[tok0:tok0 + 512, di * 128:(di + 1) * 128].rearrange("(s p) c -> p s c", p=128),
                               sg[:, :].rearrange("p (s c) -> p s c", s=4), reads=[sg.b], writes=[b_VT])
                        continue
                    for k in range(32):
                        pe.op(lambda e, k=k, bk=bk, wv=wv, x_t=x_t: e.matmul(
                            bk[:], wv[:, k, :], x_t[:, k, :], start=(k == 0), stop=(k == 31)),
                            reads=[x_t.b, wt.b], writes=[bk.b])
                    if kind == "g":
                        col = di - 32
                        act.op(lambda e, bk=bk, sg=sg, col=col: e.activation(out=sg[:], in_=bk[:], func=AF.Sigmoid,
                                                                             bias=bg[:, col:col + 1]),
                               reads=[bk.b, bg.b], writes=[sg.b])
                    elif kind == "q":
                        act.op(lambda e, bk=bk, sg=sg: e.activation(out=sg[:], in_=bk[:], func=AF.Identity,
                                                                    scale=128.0 ** -0.5),
                               reads=[bk.b], writes=[sg.b])
                    else:
                        act.op(lambda e, bk=bk, sg=sg: e.activation(out=sg[:], in_=bk[:], func=AF.Copy),
                               reads=[bk.b], writes=[sg.b])
                    if kind in ("q", "k"):
                        rb = banks[4]
                        pe.op(lambda e, sg=sg, rb=rb: e.matmul(rb[0:32, :], pm[0:32, 0:32], sg[0:32, :],
                                                               start=True, stop=True),
                              reads=[sg.b, pm.b], writes=[rb.b])
                        dve.op(lambda e, sg=sg: e.tensor_tensor(out=r2[:], in0=sg[0:32, :], in1=cos_t[:], op=ALU.mult),
                               reads=[sg.b, cos_t.b], writes=[r2.b])
                        dve.op(lambda e, rb=rb: e.tensor_tensor(out=tk[:], in0=rb[0:32, :], in1=sin_t[:], op=ALU.mult),
                               reads=[rb.b, sin_t.b], writes=[tk.b])
                        dve.op(lambda e, sg=sg: e.tensor_tensor(out=sg[0:32, :], in0=r2[:], in1=tk[:], op=ALU.add),
                               reads=[r2.b, tk.b], writes=[sg.b])
                        sp.dma(QK[di][:, tok0:tok0 + 512], sg[:], reads=[sg.b], writes=[b_QK])
                    else:
                        sp.dma(PT[di][:, tok0:tok0 + 512], sg[:], reads=[sg.b], writes=[b_PT])
        fw.barrier()

        GR = [(1, 32), (4, 8), (16, 2)]
        with ExitStack() as st:
            qT = [sbuf(st, "qT%d" % i, [128, OWN], BF16) for i in range(2)]
            kT = [sbuf(st, "kT%d" % i, [128, S_LOC], BF16) for i in range(2)]
            vh = [sbuf(st, "vh%d" % i, [128, 32, 128], BF16) for i in range(2)]
            pT = [sbuf(st, "pT%d" % i, [128, 256], BF16) for i in range(3)]
            acc = sbuf(st, "acc", [128, 2, OWN], F32)
            atb = sbuf(st, "atb", [128, OWN], BF16)
            cnt2 = {'it': 0, 'blk': 0, 'gi': 0}
            zt = sbuf(st, "zt", [128, 1024], F32)
            dve.op(lambda e: e.memset(zt[:], 0.0), writes=[zt.b])
            ztb = zt[:, :].bitcast(BF16)
            zero_jobs = []
            for e_ in range(65):
                for hf in range(2):
                    zero_jobs.append((XS[e_ * 128:(e_ + 1) * 128, hf * 2048:(hf + 1) * 2048], ztb, b_XZ))
            for hf in range(2):
                for h2 in range(2):
                    zero_jobs.append((YS[hf][64 * 128:65 * 128, h2 * 1024:(h2 + 1) * 1024], zt[:], b_YS))

            def attn_head(h):
                stages = []
                g0_last = [0]
                for g in range(3):
                    d, nb = GR[g]
                    hd = g * 8 + h
                    q_t, k_t, v_t = qT[cnt2['it'] % 2], kT[cnt2['it'] % 2], vh[cnt2['it'] % 2]
                    cnt2['it'] += 1
                    hb = nb // 2
                    lo = d * 128 * (hb - 1)
                    def loads(q_t=q_t, k_t=k_t, v_t=v_t, hd=hd, lo=lo, d=d, hb=hb, nb=nb):
                        sp.dma(q_t[:], QK[hd][:, OWN:S_LOC], writes=[q_t.b])
                        sp.dma(k_t[:, lo:S_LOC], QK[24 + hd][:, lo:S_LOC], writes=[k_t.b])
                        vsrc = VT[:, hd * 128:(hd + 1) * 128].rearrange("(n i dd) c -> dd i n c", i=128, dd=d)
                        for r in range(d):
                            sp.dma(v_t[:, r * (hb + 1):(r + 1) * (hb + 1), :], vsrc[r][:, hb - 1:nb, :],
                                   writes=[v_t.b])
                    if g < 2:
                        loads()
                    else:
                        stages[g0_last[0]] = stages[g0_last[0]][:2] + (loads,)
                    for r in range(d):
                        for n in range(hb, nb):
                            sb_, ob_ = banks[cnt2['blk'] % 2], banks[2 + cnt2['blk'] % 2]
                            p_t = pT[cnt2['blk'] % 3]
                            cnt2['blk'] += 1
                            mk = maskP if n == hb else maskR
                            qs = r + d * 128 * n - OWN
                            q_ap = q_t[:, qs:qs + d * 127 + 1:d]
                            ks0 = r + d * 128 * (n - 1)
                            ks1 = r + d * 128 * n
                            kp_ap = k_t[:, ks0:ks0 + d * 127 + 1:d]
                            kc_ap = k_t[:, ks1:ks1 + d * 127 + 1:d]
                            vi = r * (hb + 1) + (n - hb)

                            def stage_a(sb_=sb_, mk=mk, kp_ap=kp_ap, kc_ap=kc_ap, q_ap=q_ap, p_t=p_t, k_t=k_t, q_t=q_t):
                                pe.op(lambda e: e.matmul(sb_[:, 0:256], ident_b[:], mk[:], start=True, stop=False),
                                      reads=[ident_b.b, mk.b], writes=[sb_.b])
                                pe.op(lambda e: e.matmul(sb_[:, 0:128], kp_ap, q_ap, start=False, stop=False),
                                      reads=[k_t.b, q_t.b], writes=[sb_.b])
                                pe.op(lambda e: e.matmul(sb_[:, 128:256], kc_ap, q_ap, start=False, stop=True),
                                      reads=[k_t.b, q_t.b], writes=[sb_.b])
                                act.op(lambda e: e.activation(out=p_t[:], in_=sb_[:, 0:256], func=AF.Exp),
                                       reads=[sb_.b], writes=[p_t.b])

                            def stage_b(ob_=ob_, v_t=v_t, vi=vi, p_t=p_t, qs=qs, d=d, g=g):
                                pe.op(lambda e: e.matmul(ob_[:, 0:128], v_t[:, vi, :], p_t[:, 0:128], start=True, stop=False),
                                      reads=[v_t.b, p_t.b], writes=[ob_.b])
                                pe.op(lambda e: e.matmul(ob_[:, 0:128], v_t[:, vi + 1, :], p_t[:, 128:256], start=False, stop=True),
                                      reads=[v_t.b, p_t.b], writes=[ob_.b])
                                pe.op(lambda e: e.matmul(ob_[:, 128:256], ones_b[:], p_t[:, 0:128], start=True, stop=False),
                                      reads=[ones_b.b, p_t.b], writes=[ob_.b])
                                pe.op(lambda e: e.matmul(ob_[:, 128:256], ones_b[:], p_t[:, 128:256], start=False, stop=True),
                                      reads=[ones_b.b, p_t.b], writes=[ob_.b])
                                a_ap = acc[:, :, qs:qs + d * 127 + 1:d]
                                o_ap = ob_[:, 0:256].rearrange("p (a b) -> p a b", a=2)
                                if g == 0:
                                    dve.op(lambda e: e.tensor_copy(out=a_ap, in_=o_ap), reads=[ob_.b], writes=[acc.b])
                                else:
                                    dve.op(lambda e: e.tensor_tensor(out=a_ap, in0=a_ap, in1=o_ap, op=ALU.add),
                                           reads=[ob_.b, acc.b], writes=[acc.b])
                            stages.append((stage_a, stage_b, None))
                    if g == 0:
                        g0_last[0] = len(stages) - 1
                stages[0][0]()
                for i in range(len(stages)):
                    if i + 1 < len(stages):
                        stages[i + 1][0]()
                    stages[i][1]()
                    if stages[i][2] is not None:
                        stages[i][2]()
                dve.op(lambda e: e.reciprocal(out=acc[:, 1, :], in_=acc[:, 1, :]), reads=[acc.b], writes=[acc.b])
                dve.op(lambda e: e.tensor_tensor(out=atb[:], in0=acc[:, 0, :], in1=acc[:, 1, :], op=ALU.mult),
                       reads=[acc.b], writes=[atb.b])
                pool.dma(AT[h], atb[:], reads=[atb.b], writes=[b_AT])


            cw = sbuf(st, "cw", [128, 64], F32)
            cb = sbuf(st, "cb", [128, 16], F32)
            bra = sbuf(st, "bra", [128, 16], F32)
            brx = sbuf(st, "brx", [128, 16], F32)
            lm = sbuf(st, "lm", [128, 16], F32)
            spl = sbuf(st, "spl", [128, 16], F32)
            spl2 = sbuf(st, "spl2", [128, 16], F32)
            wga = sbuf(st, "wga", [128, 2, 256], BF16)
            wgx = sbuf(st, "wgx", [128, 2, 256], BF16)
            rx = sbuf(st, "rx", [128, 2, S_LOC], BF16)
            xr = [sbuf(st, "xr%d" % i, [128, S_LOC], F32) for i in range(2)]
            xrb = sbuf(st, "xrb", [128, 2, S_LOC], BF16)
            rr = sbuf(st, "rr", [128, S_LOC], F32)
            ii = sbuf(st, "ii", [128, S_LOC], F32)
            tt = sbuf(st, "tt", [128, S_LOC], F32)
            rgb = sbuf(st, "rgb", [128, OWN], BF16)
            z1 = sbuf(st, "z1", [128, OWN], F32)
            z2 = sbuf(st, "z2", [128, OWN], F32)
            recb = sbuf(st, "recb", [128, OWN], BF16)
            sp.dma(cw[:], convw, writes=[cw.b])
            sp.dma(cb[:], convb, writes=[cb.b])
            sp.dma(bra[:], brga, writes=[bra.b])
            sp.dma(brx[:], brgx, writes=[brx.b])
            sp.dma(lm[:], lam, writes=[lm.b])
            act.op(lambda e: e.activation(out=spl[:], in_=lm[:], func=AF.Exp, scale=-1.0), reads=[lm.b], writes=[spl.b])
            act.op(lambda e: e.activation(out=spl[:], in_=spl[:], func=AF.Ln, bias=1.0), reads=[spl.b], writes=[spl.b])
            dve.op(lambda e: e.tensor_scalar(out=spl2[:], in0=spl[:], scalar1=-16.0, scalar2=None, op0=ALU.mult),
                   reads=[spl.b], writes=[spl2.b])
            dve.op(lambda e: e.tensor_scalar(out=spl[:], in0=spl[:], scalar1=-8.0, scalar2=None, op0=ALU.mult),
                   reads=[spl.b], writes=[spl.b])

            def lru_block(n):
                pool.dma(wga[:], wrga[n].rearrange("p (i j) -> p i j", i=2), writes=[wga.b])
                pool.dma(wgx[:], wrgx[n].rearrange("p (i j) -> p i j", i=2), writes=[wgx.b])
                sp.dma(rx[:], PT[2 * n:2 * n + 2].rearrange("c p t -> p c t"), writes=[rx.b])
                for ic in range(2):
                    c = 2 * n + ic
                    x_ = xr[ic]
                    cv = dve
                    cv.op(lambda e, x_=x_, ic=ic, c=c: e.tensor_scalar(out=x_[:], in0=rx[:, ic, :], scalar1=cw[:, c * 4 + 3:c * 4 + 4],
                                                                       scalar2=cb[:, c:c + 1], op0=ALU.mult, op1=ALU.add),
                           reads=[rx.b, cw.b, cb.b], writes=[x_.b])
                    for k in range(3):
                        s = 3 - k
                        cv.op(lambda e, x_=x_, ic=ic, c=c, k=k, s=s: e.scalar_tensor_tensor(
                            out=x_[:, s:S_LOC], in0=rx[:, ic, 0:S_LOC - s], scalar=cw[:, c * 4 + k:c * 4 + k + 1],
                            in1=x_[:, s:S_LOC], op0=ALU.mult, op1=ALU.add), reads=[rx.b, cw.b, x_.b], writes=[x_.b])
                    act.op(lambda e, x_=x_, ic=ic: e.activation(out=xrb[:, ic, :], in_=x_[:], func=AF.Copy),
                           reads=[x_.b], writes=[xrb.b])
                for jc in range(2):
                    c = 2 * n + jc
                    x_ = xr[jc]
                    sp.dma(rgb[:], PT[16 + c][:, OWN:S_LOC], writes=[rgb.b])
                    act.op(lambda e: e.activation(out=z1[:], in_=rgb[:], func=AF.Copy), reads=[rgb.b], writes=[z1.b])
                    pool.op(lambda e: e.tensor_tensor(out=z2[:], in0=z1[:], in1=z1[:], op=ALU.mult), reads=[z1.b], writes=[z2.b])
                    pool.op(lambda e: e.tensor_scalar(out=z2[:], in0=z2[:], scalar1=0.044715, scalar2=1.0, op0=ALU.mult, op1=ALU.add),
                            reads=[z2.b], writes=[z2.b])
                    dve.op(lambda e: e.tensor_tensor(out=z2[:], in0=z2[:], in1=z1[:], op=ALU.mult), reads=[z2.b, z1.b], writes=[z2.b])
                    act.op(lambda e: e.activation(out=z2[:], in_=z2[:], func=AF.Sigmoid, scale=1.5957691216057308),
                           reads=[z2.b], writes=[z2.b])
                    dve.op(lambda e: e.tensor_tensor(out=z2[:], in0=z2[:], in1=z1[:], op=ALU.mult), reads=[z2.b, z1.b], writes=[z2.b])
                    for (wgt, bias_t, dst) in ((wga, bra, rr), (wgx, brx, ii)):
                        for tl in range(8):
                            bk = banks[4 + cnt2['gi'] % 4]
                            cnt2['gi'] += 1
                            for ic in range(2):
                                pe.op(lambda e, bk=bk, wgt=wgt, ic=ic, jc=jc, tl=tl: e.matmul(
                                    bk[:], wgt[:, ic, jc * 128:(jc + 1) * 128], xrb[:, ic, tl * 512:(tl + 1) * 512],
                                    start=(ic == 0), stop=(ic == 1)), reads=[wgt.b, xrb.b], writes=[bk.b])
                            act.op(lambda e, bk=bk, dst=dst, bias_t=bias_t, c=c, tl=tl: e.activation(
                                out=dst[:, tl * 512:(tl + 1) * 512], in_=bk[:], func=AF.Sigmoid, bias=bias_t[:, c:c + 1]),
                                reads=[bk.b, bias_t.b], writes=[dst.b])
                    act.op(lambda e, c=c: e.activation(out=tt[:], in_=rr[:], func=AF.Exp, scale=spl2[:, c:c + 1]),
                           reads=[rr.b, spl2.b], writes=[tt.b])
                    act.op(lambda e, c=c: e.activation(out=rr[:], in_=rr[:], func=AF.Exp, scale=spl[:, c:c + 1]),
                           reads=[rr.b, spl.b], writes=[rr.b])
                    act.op(lambda e: e.activation(out=tt[:], in_=tt[:], func=AF.Sqrt, scale=-1.0, bias=1.0),
                           reads=[tt.b], writes=[tt.b])
                    dve.op(lambda e, x_=x_: e.tensor_tensor(out=ii[:], in0=ii[:], in1=x_[:], op=ALU.mult),
                           reads=[ii.b, x_.b], writes=[ii.b])
                    dve.op(lambda e: e.scalar_tensor_tensor(out=ii[:, 0:OWN], in0=ii[:, 0:OWN], scalar=flag[:, 0:1],
                                                            in1=tt[:, 0:OWN], op0=ALU.mult, op1=ALU.mult),
                           reads=[ii.b, tt.b, flag.b], writes=[ii.b])
                    dve.op(lambda e: e.tensor_tensor(out=ii[:, OWN:S_LOC], in0=ii[:, OWN:S_LOC], in1=tt[:, OWN:S_LOC], op=ALU.mult),
                           reads=[ii.b, tt.b], writes=[ii.b])
                    dve.op(lambda e: e.tensor_tensor_scan(out=tt[:], data0=rr[:], data1=ii[:], initial=0.0,
                                                          op0=ALU.mult, op1=ALU.add), reads=[rr.b, ii.b], writes=[tt.b])
                    dve.op(lambda e: e.tensor_tensor(out=recb[:], in0=z2[:], in1=tt[:, OWN:S_LOC], op=ALU.mult),
                           reads=[z2.b, tt.b], writes=[recb.b])
                    pool.dma(RT[c], recb[:], reads=[recb.b], writes=[b_RT])
            for h in range(8):
                attn_head(h)
                lru_block(h)
                for i in range(4 * h, 4 * h + 4):
                    pool.dma(WPB[i], wpr[i], writes=[b_AT])
                    pool.dma(WOB[i], wout[i], writes=[b_AT])
                for (zo, zi, zb) in zero_jobs[h * 17:(h + 1) * 17]:
                    sp.dma(zo, zi, reads=[zt.b], writes=[zb])
        fw.barrier()

        def layer_norm_gen(res, gt, bt, junk, st1, st2, badd=None):
            badd = badd or pool
            dve.op(lambda e: e.reduce_sum(out=st1[:, 0:1], in_=res[:], axis=AX.X), reads=[res.b], writes=[st1.b])
            dve.op(lambda e: e.tensor_scalar(out=st1[:, 1:2], in0=st1[:, 0:1], scalar1=-1.0 / D, scalar2=None, op0=ALU.mult),
                   reads=[st1.b], writes=[st1.b])
            yield
            act.op(lambda e: e.activation(out=junk[:], in_=res[:], func=AF.Square, bias=st1[:, 1:2], accum_out=st2[:, 0:1]),
                   reads=[res.b, st1.b], writes=[junk.b, st2.b])
            act.op(lambda e: e.activation(out=st2[:, 1:2], in_=st2[:, 0:1], func=AF.Sqrt, scale=1.0 / D, bias=EPS),
                   reads=[st2.b], writes=[st2.b])
            yield
            dve.op(lambda e: e.reciprocal(out=st2[:, 2:3], in_=st2[:, 1:2]), reads=[st2.b], writes=[st2.b])
            dve.op(lambda e: e.tensor_tensor(out=st2[:, 3:4], in0=st1[:, 1:2], in1=st2[:, 2:3], op=ALU.mult),
                   reads=[st1.b, st2.b], writes=[st2.b])
            act.op(lambda e: e.activation(out=res[:], in_=res[:], func=AF.Identity, scale=st2[:, 2:3], bias=st2[:, 3:4]),
                   reads=[res.b, st2.b], writes=[res.b])
            yield
            dve.op(lambda e: e.tensor_tensor(out=res[:], in0=res[:], in1=gt[:], op=ALU.mult), reads=[res.b, gt.b], writes=[res.b])
            badd.op(lambda e: e.tensor_tensor(out=res[:], in0=res[:], in1=bt[:], op=ALU.add), reads=[res.b, bt.b], writes=[res.b])
            yield

        def layer_norm(res, gt, bt, junk, st1, st2, badd=None):
            for _ in layer_norm_gen(res, gt, bt, junk, st1, st2, badd):
                pass

        with ExitStack() as st:
            ring = Ring(st, 5, "r3_")
            G1 = sbuf(st, "G1", [128, D], F32)
            B1 = sbuf(st, "B1", [128, D], F32)
            wr_s = sbuf(st, "wr_s", [128, 32, 72], F32)
            br_s = sbuf(st, "br_s", [128, 72], F32)
            att = sbuf(st, "att", [128, 8, 256], BF16)
            rect = sbuf(st, "rect", [128, 16, 256], BF16)
            mrg = sbuf(st, "mrg", [128, 32, 256], BF16)
            gts = [sbuf(st, "gts%d" % i, [128, 2, 256], BF16) for i in range(2)]
            t1 = sbuf(st, "t1", [128, 256], F32)
            t2 = sbuf(st, "t2", [128, 256], F32)
            res = [[sbuf(st, "res%d_%d" % (a, i), [128, D], F32) for i in range(2)] for a in range(2)]
            h1bs = [sbuf(st, "h1b%d" % i, [128, D], BF16) for i in range(2)]
            h1Tg = [sbuf(st, "h1Tg%d" % i, [128, 4, 128], F32) for i in range(2)]
            st1 = sbuf(st, "st1", [128, 2], F32)
            st2 = sbuf(st, "st2", [128, 4], F32)
            lgt = sbuf(st, "lgt", [128, 72], F32)
            sm = sbuf(st, "sm", [128, 16], F32)
            ohg = sbuf(st, "ohg", [128, 8], F32)
            tmp64 = sbuf(st, "tmp64", [128, 64], F32)
            les = sbuf(st, "les", [128, 8], F32)
            le2 = sbuf(st, "le2", [128, 8], F32)
            oh1 = sbuf(st, "oh1", [128, 8], F32)
            oh2 = sbuf(st, "oh2", [128, 8], F32)
            OH = [sbuf(st, "OH%d" % i, [128, 64], F32) for i in range(2)]
            Mb = sbuf(st, "Mb", [128, 64], BF16)
            runc = sbuf(st, "runc", [128, 64], F32)
            pb = sbuf(st, "pb", [128, 64], F32)
            dst_f = sbuf(st, "dst_f", [128, 4], F32)

            sp.dma(G1[:], ln1g[0:1, :].partition_broadcast(128), writes=[G1.b])
            sp.dma(B1[:], ln1b[0:1, :].partition_broadcast(128), writes=[B1.b])
            sp.dma(wr_s[:], wrt.rearrange("p (k c) -> p k c", k=32), writes=[wr_s.b])
            sp.dma(br_s[:], brt[0:1, :].partition_broadcast(128), writes=[br_s.b])
            dve.op(lambda e: e.memset(runc[:], 0.0), writes=[runc.b])

            def epi_a(rs, sub):
                h1b = h1bs[sub % 2]
                for _ in layer_norm_gen(rs, G1, B1, h1b, st1, st2):
                    yield
                sp.dma(H1[sub * 128:(sub + 1) * 128, :], rs[:], reads=[rs.b], writes=[b_H1])
                act.op(lambda e: e.activation(out=h1b[:], in_=rs[:], func=AF.Copy), reads=[rs.b], writes=[h1b.b])
                yield

            def epi_bc(rs, sub):
                h1b = h1bs[sub % 2]
                lb = banks[3]
                for k4 in range(8):
                    hg = h1Tg[k4 % 2]
                    tb = banks[k4 % 3]
                    for i in range(4):
                        k = k4 * 4 + i
                        pe.op(lambda e, k=k, i=i, tb=tb: e.transpose(tb[:, i * 128:(i + 1) * 128], rs[:, k * 128:(k + 1) * 128], ident_f[:]),
                              reads=[rs.b, ident_f.b], writes=[tb.b])
                    act.op(lambda e, hg=hg, tb=tb: e.activation(out=hg[:], in_=tb[:, :].rearrange("p (a b) -> p a b", a=4), func=AF.Copy),
                           reads=[tb.b], writes=[hg.b])
                    for i in range(4):
                        k = k4 * 4 + i
                        pe.op(lambda e, k=k, i=i, hg=hg: e.matmul(lb[:, 0:72], hg[:, i, :], wr_s[:, k, :], start=(k == 0), stop=(k == 31)),
                              reads=[hg.b, wr_s.b], writes=[lb.b])
                    yield
                dve.op(lambda e: e.tensor_tensor(out=lgt[:], in0=lb[:, 0:72], in1=br_s[:], op=ALU.add),
                       reads=[lb.b, br_s.b], writes=[lgt.b])
                dve.op(lambda e: e.reduce_max(out=sm[:, 0:1], in_=lgt[:, 0:8], axis=AX.X), reads=[lgt.b], writes=[sm.b])
                dve.op(lambda e: e.tensor_scalar(out=ohg[:], in0=lgt[:, 0:8], scalar1=sm[:, 0:1], scalar2=None, op0=ALU.is_equal),
                       reads=[lgt.b, sm.b], writes=[ohg.b])
                dve.op(lambda e: e.tensor_scalar(out=sm[:, 1:2], in0=sm[:, 0:1], scalar1=-1.0, scalar2=None, op0=ALU.mult),
                       reads=[sm.b], writes=[sm.b])
                yield
                act.op(lambda e: e.activation(out=les[:], in_=lgt[:, 0:8], func=AF.Exp, bias=sm[:, 1:2], accum_out=sm[:, 2:3]),
                       reads=[lgt.b, sm.b], writes=[les.b, sm.b])
                dve.op(lambda e: e.reciprocal(out=sm[:, 3:4], in_=sm[:, 2:3]), reads=[sm.b], writes=[sm.b])
                dve.op(lambda e: e.tensor_tensor(out=tmp64[:, :].rearrange("p (g j) -> p g j", g=8),
                                                 in0=lgt[:, 8:72].rearrange("p (g j) -> p g j", g=8),
                                                 in1=ohg[:, :].unsqueeze(2).broadcast_to([128, 8, 8]), op=ALU.mult),
                       reads=[lgt.b, ohg.b], writes=[tmp64.b])
                dve.op(lambda e: e.reduce_sum(out=les[:], in_=tmp64[:, :].rearrange("p (g j) -> p j g", g=8), axis=AX.X),
                       reads=[tmp64.b], writes=[les.b])
                yield
                dve.op(lambda e: e.reduce_max(out=sm[:, 4:5], in_=les[:], axis=AX.X), reads=[les.b], writes=[sm.b])
                dve.op(lambda e: e.tensor_scalar(out=oh1[:], in0=les[:], scalar1=sm[:, 4:5], scalar2=None, op0=ALU.is_equal),
                       reads=[les.b, sm.b], writes=[oh1.b])
                dve.op(lambda e: e.scalar_tensor_tensor(out=le2[:], in0=oh1[:], scalar=-1e30, in1=les[:], op0=ALU.mult, op1=ALU.add),
                       reads=[oh1.b, les.b], writes=[le2.b])
                dve.op(lambda e: e.reduce_max(out=sm[:, 5:6], in_=le2[:], axis=AX.X), reads=[le2.b], writes=[sm.b])
                yield
                dve.op(lambda e: e.tensor_scalar(out=oh2[:], in0=le2[:], scalar1=sm[:, 5:6], scalar2=None, op0=ALU.is_equal),
                       reads=[le2.b, sm.b], writes=[oh2.b])
                dve.op(lambda e: e.tensor_scalar(out=sm[:, 6:7], in0=sm[:, 4:5], scalar1=-1.0, scalar2=None, op0=ALU.mult),
                       reads=[sm.b], writes=[sm.b])
                act.op(lambda e: e.activation(out=sm[:, 7:8], in_=sm[:, 5:6], func=AF.Exp, bias=sm[:, 6:7]),
                       reads=[sm.b], writes=[sm.b])
                dve.op(lambda e: e.tensor_scalar(out=sm[:, 8:9], in0=sm[:, 7:8], scalar1=1.0, scalar2=None, op0=ALU.add),
                       reads=[sm.b], writes=[sm.b])
                yield
                dve.op(lambda e: e.reciprocal(out=sm[:, 9:10], in_=sm[:, 8:9]), reads=[sm.b], writes=[sm.b])
                dve.op(lambda e: e.tensor_tensor(out=sm[:, 10:11], in0=sm[:, 9:10], in1=sm[:, 7:8], op=ALU.mult),
                       reads=[sm.b], writes=[sm.b])
                dve.op(lambda e: e.tensor_tensor(out=wts[:, 2 * sub:2 * sub + 2], in0=sm[:, 9:11],
                                                 in1=sm[:, 3:4].broadcast_to([128, 2]), op=ALU.mult),
                       reads=[sm.b], writes=[wts.b])
                for kk, oh in enumerate((oh1, oh2)):
                    dve.op(lambda e, kk=kk, oh=oh: e.tensor_tensor(
                        out=OH[kk][:, :].rearrange("p (g j) -> p g j", g=8),
                        in0=ohg[:, :].unsqueeze(2).broadcast_to([128, 8, 8]),
                        in1=oh[:, :].unsqueeze(1).broadcast_to([128, 8, 8]), op=ALU.mult),
                        reads=[ohg.b, oh.b], writes=[OH[kk].b])
                yield
                dve.op(lambda e: e.tensor_tensor(out=Mb[:], in0=OH[0][:], in1=OH[1][:], op=ALU.add),
                       reads=[OH[0].b, OH[1].b], writes=[Mb.b])
                pe.op(lambda e: e.matmul(lb[:, 128:192], ustr[:], Mb[:], start=True, stop=True),
                      reads=[ustr.b, Mb.b], writes=[lb.b])
                pe.op(lambda e: e.matmul(lb[:, 192:256], ones_b[:], Mb[:], start=True, stop=True),
                      reads=[ones_b.b, Mb.b], writes=[lb.b])
                dve.op(lambda e: e.tensor_tensor(out=pb[:], in0=lb[:, 128:192], in1=runc[:], op=ALU.add),
                       reads=[lb.b, runc.b], writes=[pb.b])
                dve.op(lambda e: e.tensor_tensor(out=runc[:], in0=lb[:, 192:256], in1=runc[:], op=ALU.add),
                       reads=[lb.b, runc.b], writes=[runc.b])
                yield
                for kk in range(2):
                    dve.op(lambda e, kk=kk: e.tensor_tensor(out=tmp64[:], in0=OH[kk][:], in1=pb[:], op=ALU.mult),
                           reads=[OH[kk].b, pb.b], writes=[tmp64.b])
                    dve.op(lambda e: e.reduce_sum(out=dst_f[:, 0:1], in_=tmp64[:], axis=AX.X), reads=[tmp64.b], writes=[dst_f.b])
                    dve.op(lambda e, kk=kk: e.tensor_tensor(out=tmp64[:], in0=OH[kk][:], in1=e128[:], op=ALU.mult),
                           reads=[OH[kk].b, e128.b], writes=[tmp64.b])
                    dve.op(lambda e: e.reduce_sum(out=dst_f[:, 1:2], in_=tmp64[:], axis=AX.X), reads=[tmp64.b], writes=[dst_f.b])
                    yield
                    dve.op(lambda e: e.tensor_scalar(out=dst_f[:, 2:3], in0=dst_f[:, 0:1], scalar1=128.0, scalar2=None, op0=ALU.is_ge),
                           reads=[dst_f.b], writes=[dst_f.b])
                    dve.op(lambda e: e.tensor_tensor(out=dst_f[:, 1:2], in0=dst_f[:, 1:2], in1=dst_f[:, 0:1], op=ALU.add),
                           reads=[dst_f.b], writes=[dst_f.b])
                    dve.op(lambda e: e.tensor_scalar(out=dst_f[:, 3:4], in0=dst_f[:, 2:3], scalar1=-1.0, scalar2=1.0, op0=ALU.mult, op1=ALU.add),
                           reads=[dst_f.b], writes=[dst_f.b])
                    dve.op(lambda e: e.tensor_tensor(out=dst_f[:, 1:2], in0=dst_f[:, 1:2], in1=dst_f[:, 3:4], op=ALU.mult),
                           reads=[dst_f.b], writes=[dst_f.b])
                    yield
                    dve.op(lambda e: e.scalar_tensor_tensor(out=dst_f[:, 1:2], in0=dst_f[:, 2:3], scalar=dumpc[:, 0:1], in1=dst_f[:, 1:2],
                                                            op0=ALU.mult, op1=ALU.add), reads=[dst_f.b, dumpc.b], writes=[dst_f.b])
                    col = 2 * sub + kk
                    dve.op(lambda e, col=col: e.tensor_tensor(out=wts[:, col:col + 1], in0=wts[:, col:col + 1], in1=dst_f[:, 3:4], op=ALU.mult),
                           reads=[dst_f.b, wts.b], writes=[wts.b])
                    dve.op(lambda e, col=col: e.tensor_copy(out=destI[:, col:col + 1], in_=dst_f[:, 1:2]),
                           reads=[dst_f.b], writes=[destI.b])
                    pool.op(lambda e, col=col: e.indirect_dma_start(
                        out=XS[:, :], out_offset=bass.IndirectOffsetOnAxis(ap=destI[:, col:col + 1], axis=0),
                        in_=h1b[:, :], in_offset=None),
                        reads=[h1b.b, destI.b, b_XZ], writes=[b_XS], is_dma=True)
                    yield

            epi = [iter(())]
            epi_ln = [iter(())]

            def step(n=1, g=None):
                g = g or epi
                for _ in range(n):
                    try:
                        next(g[0])
                    except StopIteration:
                        return

            def drain():
                for _ in epi_ln[0]:
                    pass
                for _ in epi[0]:
                    pass

            def chain2(a, b):
                for _ in a:
                    yield
                for _ in b:
                    yield

            gcnt = 0
            for tile in range(8):
                t0 = tile * 256
                rs2 = res[tile % 2]
                sp.dma(att[:], AT[:, :, t0:t0 + 256].rearrange("c p t -> p c t"), writes=[att.b])
                sp.dma(rect[:], RT[:, :, t0:t0 + 256].rearrange("c p t -> p c t"), writes=[rect.b])
                for s_ in range(2):
                    sp.dma(rs2[s_][:], xown[t0 + s_ * 128:t0 + (s_ + 1) * 128, :], writes=[rs2[s_].b])
                for j in range(32):
                    pa = ring.load(WPB[j], 3072)
                    pav = pa[:, 0:1024].rearrange("p (k c) -> p k c", k=8)
                    prv = pa[:, 1024:3072].rearrange("p (k c) -> p k c", k=16)
                    yb_ = banks[gcnt % 2]
                    gt = gts[gcnt % 2]
                    gcnt += 1
                    pool.dma(gt[:], PT[32 + j:96:32][:, :, OWN + t0:OWN + t0 + 256].rearrange("g p t -> p g t"),
                             writes=[gt.b])
                    for k in range(8):
                        pe.op(lambda e, yb_=yb_, pav=pav, k=k: e.matmul(yb_[:, 0:256], pav[:, k, :], att[:, k, :], start=(k == 0), stop=(k == 7)),
                              reads=[pa.b, att.b], writes=[yb_.b])
                    for k in range(16):
                        pe.op(lambda e, yb_=yb_, prv=prv, k=k: e.matmul(yb_[:, 256:512], prv[:, k, :], rect[:, k, :], start=(k == 0), stop=(k == 15)),
                              reads=[pa.b, rect.b], writes=[yb_.b])
                    dve.op(lambda e, gt=gt, yb_=yb_: e.tensor_tensor(out=t1[:], in0=gt[:, 0, :], in1=yb_[:, 0:256], op=ALU.mult),
                           reads=[gt.b, yb_.b], writes=[t1.b])
                    dve.op(lambda e, gt=gt, yb_=yb_: e.tensor_tensor(out=t2[:], in0=gt[:, 1, :], in1=yb_[:, 256:512], op=ALU.mult),
                           reads=[gt.b, yb_.b], writes=[t2.b])
                    dve.op(lambda e, j=j: e.tensor_tensor(out=mrg[:, j, :], in0=t1[:], in1=t2[:], op=ALU.add),
                           reads=[t1.b, t2.b], writes=[mrg.b])
                    if j % 3 == 2:
                        step(1, epi_ln)
                for _ in epi_ln[0]:
                    pass
                for eg in range(8):
                    for q in range(4):
                        po = ring.load(WOB[eg * 4 + q])
                        pov = po[:, :].rearrange("p (k e) -> p k e", k=8)
                        for s_ in range(2):
                            bk = banks[4 + 2 * (eg % 2) + s_]
                            for kk in range(8):
                                pe.op(lambda e, bk=bk, pov=pov, q=q, kk=kk, s_=s_: e.matmul(
                                    bk[:], mrg[:, q * 8 + kk, s_ * 128:(s_ + 1) * 128], pov[:, kk, :],
                                    start=(q == 0 and kk == 0), stop=(q == 3 and kk == 7)),
                                    reads=[po.b, mrg.b], writes=[bk.b])
                        step(2 if (eg * 4 + q) % 4 == 0 else 1)
                    for s_ in range(2):
                        bk = banks[4 + 2 * (eg % 2) + s_]
                        rs = rs2[s_]
                        dve.op(lambda e, bk=bk, rs=rs, eg=eg: e.scalar_tensor_tensor(
                            out=rs[:, eg * 512:(eg + 1) * 512], in0=rs[:, eg * 512:(eg + 1) * 512], scalar=ALPHA,
                            in1=bk[:], op0=ALU.mult, op1=ALU.add), reads=[bk.b, rs.b], writes=[rs.b])
                drain()
                epi_ln[0] = chain2(epi_a(rs2[0], tile * 2), epi_a(rs2[1], tile * 2 + 1))
                epi[0] = chain2(epi_bc(rs2[0], tile * 2), epi_bc(rs2[1], tile * 2 + 1))
            drain()
        fw.barrier()

        with ExitStack() as st:
            ring = Ring(st, 14, "r4_")
            xe = [sbuf(st, "xe%d" % i, [128, D], BF16) for i in range(2)]
            xeT = [sbuf(st, "xeT%d" % i, [128, 32, 128], BF16) for i in range(2)]
            sg_t = sbuf(st, "sg_t", [128, 512], F32)
            hid = [sbuf(st, "hid%d" % i, [128, 512], BF16) for i in range(2)]
            ye = [sbuf(st, "ye%d" % i, [128, D], F32) for i in range(2)]
            ev = 0
            for e_ in range(64):
                x_e, x_T, hd_, y_e = xe[e_ % 2], xeT[e_ % 2], hid[e_ % 2], ye[e_ % 2]
                if e_ == 0:
                    sp.dma(x_e[:], XS[0:128, :], writes=[x_e.b])
                for k8 in range(4):
                    bk = banks[k8 % 2]
                    bkb = bk[:, :].bitcast(BF16)
                    for i in range(8):
                        k = k8 * 8 + i
                        pe.op(lambda e, bkb=bkb, x_e=x_e, k=k, i=i: e.transpose(bkb[:, i * 128:(i + 1) * 128], x_e[:, k * 128:(k + 1) * 128], ident_b[:]),
                              reads=[x_e.b, ident_b.b], writes=[bk.b])
                    act.op(lambda e, bkb=bkb, x_T=x_T, k8=k8: e.activation(
                        out=x_T[:, k8 * 8:(k8 + 1) * 8, :], in_=bkb.rearrange("p (a b) -> p a b", a=8), func=AF.Copy),
                        reads=[bk.b], writes=[x_T.b])
                gb, ub = banks[2], banks[3]
                for q in range(4):
                    pg = ring.load(wg[e_][q])
                    pu = ring.load(wu[e_][q])
                    for (pp, bk) in ((pg, gb), (pu, ub)):
                        ppv = pp[:, :].rearrange("p (k f) -> p k f", k=8)
                        for fc in range(4):
                            for kk in range(8):
                                pe.op(lambda e, bk=bk, ppv=ppv, fc=fc, kk=kk, q=q, x_T=x_T: e.matmul(
                                    bk[:, fc * 128:(fc + 1) * 128], ppv[:, kk, fc * 128:(fc + 1) * 128], x_T[:, q * 8 + kk, :],
                                    start=(q == 0 and kk == 0 and fc == 0), stop=(q == 3 and kk == 7 and fc == 3),
                                    skip_group_check=True),
                                    reads=[pp.b, x_T.b], writes=[bk.b])
                act.op(lambda e, gb=gb: e.activation(out=sg_t[:], in_=gb[:], func=AF.Silu), reads=[gb.b], writes=[sg_t.b])
                dve.op(lambda e, ub=ub, hd_=hd_: e.tensor_tensor(out=hd_[:], in0=sg_t[:], in1=ub[:], op=ALU.mult),
                       reads=[sg_t.b, ub.b], writes=[hd_.b])
                pds = [ring.load(wd[e_][fc * 128:(fc + 1) * 128, :]) for fc in range(4)]
                for dg in range(8):
                    bk = banks[4 + dg % 4]
                    for fc in range(4):
                        pe.op(lambda e, bk=bk, fc=fc, dg=dg, hd_=hd_, pd=pds[fc]: e.matmul(
                            bk[:], hd_[:, fc * 128:(fc + 1) * 128], pd[:, dg * 512:(dg + 1) * 512], start=(fc == 0), stop=(fc == 3)),
                            reads=[hd_.b, pds[fc].b], writes=[bk.b])
                    if ev % 2 == 0:
                        act.op(lambda e, bk=bk, y_e=y_e, dg=dg: e.activation(out=y_e[:, dg * 512:(dg + 1) * 512], in_=bk[:], func=AF.Copy),
                               reads=[bk.b], writes=[y_e.b])
                    else:
                        dve.op(lambda e, bk=bk, y_e=y_e, dg=dg: e.tensor_copy(out=y_e[:, dg * 512:(dg + 1) * 512], in_=bk[:]),
                               reads=[bk.b], writes=[y_e.b])
                    ev += 1
                if e_ + 1 < 64:
                    xn = xe[(e_ + 1) % 2]
                    sp.dma(xn[:], XS[(e_ + 1) * 128:(e_ + 2) * 128, :], writes=[xn.b])
                for hf in range(2):
                    sp.dma(YS[hf][e_ * 128:(e_ + 1) * 128, :], y_e[:, hf * 2048:(hf + 1) * 2048], reads=[y_e.b], writes=[b_YS])
        fw.barrier()

        with ExitStack() as st:
            G2 = sbuf(st, "G2", [128, D], F32)
            B2 = sbuf(st, "B2", [128, D], F32)
            hr = [sbuf(st, "hr%d" % i, [128, D], F32) for i in range(2)]
            ya = [sbuf(st, "ya%d" % i, [128, D], F32) for i in range(2)]
            yb = [sbuf(st, "yb%d" % i, [128, D], F32) for i in range(2)]
            junk = sbuf(st, "junk", [128, D], BF16)
            st1 = sbuf(st, "st1b", [128, 2], F32)
            st2 = sbuf(st, "st2b", [128, 4], F32)
            sp.dma(G2[:], ln2g[0:1, :].partition_broadcast(128), writes=[G2.b])
            sp.dma(B2[:], ln2b[0:1, :].partition_broadcast(128), writes=[B2.b])
            for i in range(2):
                dve.op(lambda e, i=i: e.memset(ya[i][:], 0.0), writes=[ya[i].b])
                dve.op(lambda e, i=i: e.memset(yb[i][:], 0.0), writes=[yb[i].b])
            for sub in range(16):
                h_, a_, b_ = hr[sub % 2], ya[sub % 2], yb[sub % 2]
                sp.dma(h_[:], H1[sub * 128:(sub + 1) * 128, :], writes=[h_.b])
                for (dstt, col) in ((a_, 2 * sub), (b_, 2 * sub + 1)):
                    for hf in range(2):
                        pool.op(lambda e, dstt=dstt, col=col, hf=hf: e.indirect_dma_start(
                            out=dstt[:, hf * 2048:(hf + 1) * 2048], out_offset=None, in_=YS[hf][:, :],
                            in_offset=bass.IndirectOffsetOnAxis(ap=destI[:, col:col + 1], axis=0)),
                            reads=[destI.b], writes=[dstt.b], is_dma=True)
                act.op(lambda e, h_=h_: e.activation(out=h_[:], in_=h_[:], func=AF.Identity, scale=ALPHA), reads=[h_.b], writes=[h_.b])
                dve.op(lambda e, h_=h_, a_=a_, sub=sub: e.scalar_tensor_tensor(out=h_[:], in0=a_[:], scalar=wts[:, 2 * sub:2 * sub + 1],
                                                                              in1=h_[:], op0=ALU.mult, op1=ALU.add),
                       reads=[a_.b, h_.b, wts.b], writes=[h_.b])
                dve.op(lambda e, h_=h_, b_=b_, sub=sub: e.scalar_tensor_tensor(out=h_[:], in0=b_[:], scalar=wts[:, 2 * sub + 1:2 * sub + 2],
                                                                              in1=h_[:], op0=ALU.mult, op1=ALU.add),
                       reads=[b_.b, h_.b, wts.b], writes=[h_.b])
                layer_norm(h_, G2, B2, junk, st1, st2, badd=dve)
                pool.dma(out[sub * 128:(sub + 1) * 128, :], h_[:], reads=[h_.b], writes=[b_out])

        fw.emit(top)
    return nc


def _consts(half):
    ident = np.eye(128, dtype=np.float32)
    NEG = -30000.0
    k = np.arange(128)[:, None]
    q = np.arange(128)[None, :]
    mprev = np.where(k >= q, 0.0, NEG).astype(np.float32)
    mcur = np.where(k <= q, 0.0, NEG).astype(np.float32)
    maskR = np.concatenate([mprev, mcur], axis=1)
    maskP = maskR.copy()
    if half == 0:
        maskP[:, :128] = NEG
    pm = np.zeros((32, 32), np.float32)
    for m in range(16):
        pm[m + 16, m] = -1.0
    for m in range(16, 32):
        pm[m - 16, m] = 1.0
    invf = np.power(np.float32(500000.0), -np.arange(16, dtype=np.float32) * np.float32(2.0) / np.float32(32)).astype(np.float32)
    invf = np.concatenate([invf, invf]).reshape(32, 1)
    flag = np.full((128, 1), float(half), np.float32)
    ustr = (np.arange(128)[:, None] < np.arange(128)[None, :]).astype(np.float32)
    e128 = np.tile((np.arange(64, dtype=np.float32) * 128.0)[None, :], (128, 1))
    dump = (8192.0 + np.arange(128, dtype=np.float32)).reshape(128, 1)
    return dict(c_dump=dump, c_ident=ident, c_maskR=maskR, c_maskP=maskP, c_pm=pm, c_invf=invf, c_flag=flag, c_ustr=ustr, c_e128=e128)


def _prep_shared(inp):
    f = np.float32
    sh = {}
    w_in = np.asarray(inp["w_in"], f)[0]
    sh["win"] = np.ascontiguousarray(w_in.reshape(32, 128, NCH_IN, 128).transpose(2, 1, 0, 3)).reshape(NCH_IN, 128, 4096)
    bgt = np.asarray(inp["b_gate"], f)[0]
    sh["bgate"] = np.ascontiguousarray(bgt.reshape(2, 32, 128).transpose(2, 0, 1)).reshape(128, 64)
    cw = np.asarray(inp["conv_w"], f)[0]
    sh["convw"] = np.ascontiguousarray(cw.reshape(4, 16, 128).transpose(2, 1, 0)).reshape(128, 64)
    sh["convb"] = np.ascontiguousarray(np.asarray(inp["conv_b"], f)[0].reshape(16, 128).T)
    for nm, key in (("wrga", "w_rg_a"), ("wrgx", "w_rg_x")):
        w = np.asarray(inp[key], f)[0]
        sh[nm] = np.ascontiguousarray(w.reshape(8, 2, 128, 256).transpose(0, 2, 1, 3)).reshape(8, 128, 512)
    for nm, key in (("brga", "b_rg_a"), ("brgx", "b_rg_x")):
        sh[nm] = np.ascontiguousarray(np.asarray(inp[key], f)[0].reshape(16, 128).T)
    sh["lam"] = np.ascontiguousarray(np.asarray(inp["lru_lambda"], f)[0].reshape(16, 128).T)
    wa = np.asarray(inp["w_attn_proj"], f)[0]
    wa_l = wa.reshape(8, 128, 32, 128).transpose(2, 1, 0, 3).reshape(32, 128, 1024)
    wr = np.asarray(inp["w_rec_proj"], f)[0]
    wr_l = wr.reshape(16, 128, 32, 128).transpose(2, 1, 0, 3).reshape(32, 128, 2048)
    sh["wpr"] = np.ascontiguousarray(np.concatenate([wa_l, wr_l], axis=2))
    wo = np.asarray(inp["w_out"], f)[0]
    sh["wout"] = np.ascontiguousarray(wo.reshape(4, 8, 128, 8, 512).transpose(3, 0, 2, 1, 4)).reshape(32, 128, 4096)
    sh["ln1g"] = np.asarray(inp["ln1_g"], f).reshape(1, D)
    sh["ln1b"] = np.asarray(inp["ln1_b"], f).reshape(1, D)
    wrt = np.concatenate([np.asarray(inp["w_router_group"], f)[0], np.asarray(inp["w_router_expert"], f)[0]], axis=1)
    sh["wrt"] = np.ascontiguousarray(wrt.reshape(32, 128, 72).transpose(1, 0, 2)).reshape(128, 32 * 72)
    sh["brt"] = np.concatenate([np.asarray(inp["b_router_group"], f)[0], np.asarray(inp["b_router_expert"], f)[0]]).reshape(1, 72)
    for nm, key in (("wg", "w_gate"), ("wu", "w_up")):
        w = np.asarray(inp[key], f)[0]
        sh[nm] = np.ascontiguousarray(w.reshape(64, 4, 8, 128, 512).transpose(0, 1, 3, 2, 4)).reshape(64, 4, 128, 4096)
    sh["wd"] = np.asarray(inp["w_down"], f)[0]
    sh["ln2g"] = np.asarray(inp["ln2_g"], f).reshape(1, D)
    sh["ln2b"] = np.asarray(inp["ln2_b"], f).reshape(1, D)
    return sh


def kernel(**inp):
    x = np.asarray(inp["x"], np.float32)
    positions = np.asarray(inp["positions"], np.int32)
    sh = _prep_shared(inp)
    in_maps = []
    for c in range(8):
        b, half = c // 2, c % 2
        xl = np.zeros((S_LOC, D), np.float32)
        pl = np.zeros((1, S_LOC), np.int32)
        if half == 1:
            xl[:] = x[b]
            pl[0] = positions[b]
        else:
            xl[OWN:] = x[b, :OWN]
            pl[0, OWN:] = positions[b, :OWN]
        xT = np.ascontiguousarray(xl.reshape(8, 512, 32, 128).transpose(0, 3, 2, 1)).reshape(8, 128, 32 * 512)
        m = dict(sh)
        m["xT"] = xT
        m["xown"] = np.ascontiguousarray(x[b, half * OWN:(half + 1) * OWN])
        m["pos"] = pl
        m.update(_consts(half))
        in_maps.append(m)
    nc = build_nc()
    res = run_bass_kernel_spmd(nc, in_maps, core_ids=list(range(8)))
    outp = np.zeros((4, 4096, D), np.float32)
    for c in range(8):
        b, half = c // 2, c % 2
        outp[b, half * OWN:(half + 1) * OWN] = np.asarray(res.results[c]["out"], np.float32)
    return outp
```

```python
import math
from contextlib import ExitStack
import numpy as np
import concourse.bass as bass
import concourse.mybir as mybir
from concourse.bass_utils import run_bass_kernel_spmd

F32 = mybir.dt.float32
BF16 = mybir.dt.bfloat16
I32 = mybir.dt.int32
AF = mybir.ActivationFunctionType
ALU = mybir.AluOpType
AX = mybir.AxisListType

SAME_ENGINE_SYNC = True
N_DMA_SEMS = 24

S_LOC = 4096
OWN = 2048
D = 4096
NCH_IN = 168
ALPHA = 2.0 ** 0.25
EPS = 1e-5
MAGIC = 12582912.0
TWO_PI = 2.0 * math.pi


class Buf:
    __slots__ = ("name", "w", "r", "multi", "ws")

    def __init__(self, name="", multi=False):
        self.name = name
        self.w = None
        self.r = {}
        self.multi = multi
        self.ws = []


class Op:
    __slots__ = ("eng", "fn", "deps", "sig", "sigval", "is_dma", "dsem", "dval")

    def __init__(self, eng, fn, is_dma):
        self.eng = eng
        self.fn = fn
        self.deps = []
        self.sig = False
        self.sigval = None
        self.is_dma = is_dma
        self.dsem = None
        self.dval = None


class Eng:
    def __init__(self, fw, name):
        self.fw = fw
        self.name = name
        self.ops = []
        self.sem = None
        self.dsems = []
        self.pending = []

    def op(self, fn, reads=(), writes=(), is_dma=False):
        o = Op(self, fn, is_dma)
        deps = list(self.pending)
        self.pending = []
        for b in reads:
            if b.multi:
                deps.extend(b.ws)
            elif b.w is not None:
                deps.append(b.w)
        for b in writes:
            if b.multi:
                continue
            if b.w is not None:
                deps.append(b.w)
            deps.extend(b.r.values())
        seen = set()
        for d in deps:
            if d is o or id(d) in seen:
                continue
            seen.add(id(d))
            if (not d.is_dma) and d.eng is self and not is_dma:
                if self.name == "pe" or not SAME_ENGINE_SYNC:
                    continue
            d.sig = True
            o.deps.append(d)
        key = (self.name, is_dma)
        for b in reads:
            if b.multi:
                continue
            if is_dma:
                b.r[(key, len(b.r))] = o
            else:
                b.r[key] = o
        for b in writes:
            if b.multi:
                b.ws.append(o)
                continue
            b.w = o
            b.r = {}
        self.ops.append(o)
        self.fw.all_ops.append(o)
        return o

    def dma(self, out, in_, reads=(), writes=(), **kw):
        return self.op(lambda e: e.dma_start(out=out, in_=in_, **kw), reads, writes, is_dma=True)


class FW:
    def __init__(self, nc):
        self.nc = nc
        self.all_ops = []
        self.pe = Eng(self, "pe")
        self.act = Eng(self, "act")
        self.dve = Eng(self, "dve")
        self.pool = Eng(self, "pool")
        self.sp = Eng(self, "sp")
        self.engs = [self.pe, self.act, self.dve, self.pool, self.sp]
        self.mark = 0

    def barrier(self):
        lasts = []
        for e in self.engs:
            last_c = None
            for o in reversed(e.ops):
                if not o.is_dma:
                    last_c = o
                    break
            if last_c is not None:
                lasts.append(last_c)
        dm = []
        for e in self.engs:
            dl = [o for o in e.ops if o.is_dma]
            dm.extend(dl[-N_DMA_SEMS:])
        for e in self.engs:
            e.pending = list(e.pending) + lasts + dm

    def emit(self, stack):
        nc = self.nc
        for e in self.engs:
            e.sem = stack.enter_context(nc.semaphore("s_" + e.name))
            if e.name in ("sp", "act", "pool"):
                e.dsems = [stack.enter_context(nc.semaphore("d_%s_%d" % (e.name, i))) for i in range(N_DMA_SEMS)]
        for e in self.engs:
            c = 0
            k = 0
            uses = [0] * max(1, len(e.dsems))
            for o in e.ops:
                if o.is_dma:
                    s = k % len(e.dsems)
                    k += 1
                    uses[s] += 1
                    o.dsem = e.dsems[s]
                    o.dval = 16 * uses[s]
                elif o.sig:
                    c += 1
                    o.sigval = c
        block = stack.enter_context(nc.Block())

        def run(eng_obj, h):
            seen = {}

            def wait(sem, val):
                key = id(sem)
                if seen.get(key, 0) >= val:
                    return
                seen[key] = val
                h.wait_ge(sem, val)

            for o in eng_obj.ops:
                for d in o.deps:
                    if d.is_dma:
                        wait(d.dsem, d.dval)
                    else:
                        wait(d.eng.sem, d.sigval)
                if o.is_dma:
                    if o.dval > 16:
                        wait(o.dsem, o.dval - 16)
                    o.fn(h).then_inc(o.dsem, 16)
                else:
                    ins = o.fn(h)
                    if o.sig:
                        ins.then_inc(eng_obj.sem, 1)
            last = {}
            for o in eng_obj.ops:
                if o.is_dma:
                    last[id(o.dsem)] = (o.dsem, o.dval)
            for sem, val in last.values():
                wait(sem, val)

        @block.tensor
        def _(h):
            run(self.pe, h)

        @block.scalar
        def _(h):
            run(self.act, h)

        @block.vector
        def _(h):
            run(self.dve, h)

        @block.gpsimd
        def _(h):
            run(self.pool, h)

        @block.sync
        def _(h):
            run(self.sp, h)


class T:
    def __init__(self, h, name):
        self.h = h
        self.b = Buf(name)

    def __getitem__(self, k):
        return self.h[k]


def build_nc():
    nc = bass.Bass("TRN2", target_bir_lowering=False)

    def din(name, shape, dt=F32):
        return nc.dram_tensor(name, list(shape), dt, kind="ExternalInput").ap()

    def dscr(name, shape, dt):
        return nc.dram_tensor(name, list(shape), dt, kind="Internal").ap()

    xT = din("xT", [8, 128, 32 * 512])
    xown = din("xown", [OWN, D])
    pos = din("pos", [1, S_LOC], I32)
    win = din("win", [NCH_IN, 128, 4096])
    bgate = din("bgate", [128, 64])
    convw = din("convw", [128, 16 * 4])
    convb = din("convb", [128, 16])
    wrga = din("wrga", [8, 128, 512])
    wrgx = din("wrgx", [8, 128, 512])
    brga = din("brga", [128, 16])
    brgx = din("brgx", [128, 16])
    lam = din("lam", [128, 16])
    wpr = din("wpr", [32, 128, 3072])
    wout = din("wout", [32, 128, 4096])
    ln1g = din("ln1g", [1, D])
    ln1b = din("ln1b", [1, D])
    wrt = din("wrt", [128, 32 * 72])
    brt = din("brt", [1, 72])
    wg = din("wg", [64, 4, 128, 4096])
    wu = din("wu", [64, 4, 128, 4096])
    wd = din("wd", [64, 512, D])
    ln2g = din("ln2g", [1, D])
    ln2b = din("ln2b", [1, D])
    c_ident = din("c_ident", [128, 128])
    c_maskR = din("c_maskR", [128, 256])
    c_maskP = din("c_maskP", [128, 256])
    c_pm = din("c_pm", [32, 32])
    c_invf = din("c_invf", [32, 1])
    c_flag = din("c_flag", [128, 1])
    c_ustr = din("c_ustr", [128, 128])
    c_e128 = din("c_e128", [128, 64])
    c_dump = din("c_dump", [128, 1])
    out = nc.dram_tensor("out", [OWN, D], F32, kind="ExternalOutput").ap()

    QK = dscr("QK", [48, 128, S_LOC], BF16)
    VT = dscr("VT", [S_LOC, 3072], BF16)
    PT = dscr("PT", [96, 128, S_LOC], BF16)
    AT = dscr("AT", [8, 128, OWN], BF16)
    RT = dscr("RT", [16, 128, OWN], BF16)
    H1 = dscr("H1", [OWN, D], F32)
    WPB = dscr("WPB", [32, 128, 3072], BF16)
    WOB = dscr("WOB", [32, 128, 4096], BF16)
    XS = dscr("XS", [65 * 128, D], BF16)
    YS = [dscr("YS%d" % i, [65 * 128, 2048], F32) for i in range(2)]

    fw = FW(nc)
    pe, act, dve, pool, sp = fw.pe, fw.act, fw.dve, fw.pool, fw.sp
    b_QK, b_VT, b_PT, b_AT, b_RT, b_H1, b_XS, b_YS, b_out = [Buf(n, multi=True) for n in "QK VT PT AT RT H1 XS YS out".split()]
    b_XZ = Buf("XSzero", multi=True)

    with ExitStack() as top:
        def sbuf(st, name, shape, dt):
            return T(st.enter_context(nc.sbuf_tensor(name, list(shape), dt)), name)

        banks = [T(top.enter_context(nc.psum_tensor("bank%d" % i, [128, 512], F32)), "bank%d" % i) for i in range(8)]

        ident_f = sbuf(top, "ident_f", [128, 128], F32)
        ident_b = sbuf(top, "ident_b", [128, 128], BF16)
        ones_b = sbuf(top, "ones_b", [128, 128], BF16)
        maskR = sbuf(top, "maskR", [128, 256], BF16)
        maskP = sbuf(top, "maskP", [128, 256], BF16)
        pm = sbuf(top, "pm", [32, 32], BF16)
        invf = sbuf(top, "invf", [32, 1], F32)
        flag = sbuf(top, "flag", [128, 1], F32)
        ustr = sbuf(top, "ustr", [128, 128], BF16)
        e128 = sbuf(top, "e128", [128, 64], F32)
        dumpc = sbuf(top, "dumpc", [128, 1], F32)
        bg = sbuf(top, "bg", [128, 64], F32)
        destI = sbuf(top, "destI", [128, 32], I32)
        wts = sbuf(top, "wts", [128, 32], F32)

        sp.dma(ident_f[:], c_ident, writes=[ident_f.b])
        pool.dma(ident_b[:], c_ident, writes=[ident_b.b])
        pool.dma(maskR[:], c_maskR, writes=[maskR.b])
        pool.dma(maskP[:], c_maskP, writes=[maskP.b])
        pool.dma(pm[:], c_pm, writes=[pm.b])
        pool.dma(ustr[:], c_ustr, writes=[ustr.b])
        sp.dma(invf[:], c_invf, writes=[invf.b])
        sp.dma(flag[:], c_flag, writes=[flag.b])
        sp.dma(e128[:], c_e128, writes=[e128.b])
        sp.dma(dumpc[:], c_dump, writes=[dumpc.b])
        sp.dma(bg[:], bgate, writes=[bg.b])
        dve.op(lambda e: e.memset(ones_b[:], 1.0), writes=[ones_b.b])

        class Ring:
            def __init__(self, st, n, name):
                self.slots = [sbuf(st, "%s%d" % (name, i), [128, 4096], BF16) for i in range(n)]
                self.i = 0

            def load(self, src, nel=4096, k=None):
                s = self.slots[self.i % len(self.slots)]
                self.i += 1
                o = s[:, 0:nel]
                if k is not None:
                    o = o.rearrange("p (k e) -> p k e", k=k)
                pool.dma(o, src, writes=[s.b])
                return s

        with ExitStack() as st:
            ring = Ring(st, 8, "r1_")
            xt = [sbuf(st, "xt%d" % i, [128, 32, 512], BF16) for i in range(2)]
            stg = [sbuf(st, "stg%d" % i, [128, 512], BF16) for i in range(4)]
            posi = sbuf(st, "posi", [32, 512], I32)
            ang = sbuf(st, "ang", [32, 512], F32)
            tk = sbuf(st, "tk", [32, 512], F32)
            cos_t = sbuf(st, "cos_t", [32, 512], F32)
            sin_t = sbuf(st, "sin_t", [32, 512], F32)
            r1 = sbuf(st, "rp1", [32, 512], F32)
            r2 = sbuf(st, "rp2", [32, 512], F32)

            def load_xt(lt):
                t = xt[lt % 2]
                for q in range(4):
                    pool.dma(t[:, q * 8:(q + 1) * 8, :],
                             xT[lt][:, q * 4096:(q + 1) * 4096].rearrange("p (k t) -> p k t", k=8),
                             writes=[t.b])

            def trig(dst, shift):
                dve.op(lambda e: e.tensor_scalar(out=r1[:], in0=ang[:], scalar1=shift, scalar2=None, op0=ALU.add),
                       reads=[ang.b], writes=[r1.b])
                dve.op(lambda e: e.tensor_scalar(out=tk[:], in0=r1[:], scalar1=1.0 / TWO_PI, scalar2=MAGIC,
                                                 op0=ALU.mult, op1=ALU.add), reads=[r1.b], writes=[tk.b])
                dve.op(lambda e: e.tensor_scalar(out=tk[:], in0=tk[:], scalar1=-MAGIC, scalar2=None, op0=ALU.add),
                       reads=[tk.b], writes=[tk.b])
                dve.op(lambda e: e.scalar_tensor_tensor(out=r1[:], in0=tk[:], scalar=-TWO_PI, in1=r1[:],
                                                        op0=ALU.mult, op1=ALU.add), reads=[tk.b, r1.b], writes=[r1.b])
                dve.op(lambda e: e.tensor_scalar(out=r1[:], in0=r1[:], scalar1=3.14159, scalar2=-3.14159,
                                                 op0=ALU.min, op1=ALU.max), reads=[r1.b], writes=[r1.b])
                act.op(lambda e: e.activation(out=dst[:], in_=r1[:], func=AF.Sin), reads=[r1.b], writes=[dst.b])

            def chunks_for(lt):
                L = []
                own = lt >= 4
                for hd in range(24):
                    g = hd // 8
                    need_kv = own or g == 2 or lt == 3
                    if own:
                        L.append((hd, "q", hd))
                    if need_kv:
                        L.append((24 + hd, "k", 24 + hd))
                        L.append((48 + hd, "v", hd))
                for c in range(16):
                    L.append((72 + c, "p", c))
                if own:
                    for c in range(16):
                        L.append((88 + c, "p", 16 + c))
                    for c in range(64):
                        L.append((104 + c, "g", 32 + c))
                return L

            load_xt(0)
            cnt = 0
            for lt in range(8):
                x_t = xt[lt % 2]
                tok0 = lt * 512
                if lt + 1 < 8:
                    load_xt(lt + 1)
                sp.dma(posi[:], pos[0:1, tok0:tok0 + 512].partition_broadcast(32), writes=[posi.b])
                dve.op(lambda e: e.tensor_copy(out=ang[:], in_=posi[:]), reads=[posi.b], writes=[ang.b])
                dve.op(lambda e: e.tensor_scalar(out=ang[:], in0=ang[:], scalar1=invf[:, 0:1], scalar2=None, op0=ALU.mult),
                       reads=[ang.b, invf.b], writes=[ang.b])
                trig(sin_t, 0.0)
                trig(cos_t, math.pi / 2)
                for (wc, kind, di) in chunks_for(lt):
                    wt = ring.load(win[wc])
                    wv = wt[:, :].rearrange("p (k c) -> p k c", k=32)
                    bk = banks[cnt % 4]
                    sg = stg[cnt % 4]
                    cnt += 1
                    if kind == "v":
                        for s in range(4):
                            for k in range(32):
                                pe.op(lambda e, s=s, k=k, bk=bk, wv=wv, x_t=x_t: e.matmul(
                                    bk[:, s * 128:(s + 1) * 128], x_t[:, k, s * 128:(s + 1) * 128], wv[:, k, :],
                                    start=(k == 0), stop=(k == 31)),
                                    reads=[x_t.b, wt.b], writes=[bk.b])
                        act.op(lambda e, bk=bk, sg=sg: e.activation(out=sg[:], in_=bk[:], func=AF.Copy),
                               reads=[bk.b], writes=[sg.b])
                        sp.dma(VT[tok0:tok0 + 512, di * 128:(di + 1) * 128].rearrange("(s p) c -> p s c", p=128),
                               sg[:, :].rearrange("p (s c) -> p s c", s=4), reads=[sg.b], writes=[b_VT])
                        continue
                    for k in range(32):
                        pe.op(lambda e, k=k, bk=bk, wv=wv, x_t=x_t: e.matmul(
                            bk[:], wv[:, k, :], x_t[:, k, :], start=(k == 0), stop=(k == 31)),
                            reads=[x_t.b, wt.b], writes=[bk.b])
                    if kind == "g":
                        col = di - 32
                        act.op(lambda e, bk=bk, sg=sg, col=col: e.activation(out=sg[:], in_=bk[:], func=AF.Sigmoid,
                                                                             bias=bg[:, col:col + 1]),
                               reads=[bk.b, bg.b], writes=[sg.b])
                    elif kind == "q":
                        act.op(lambda e, bk=bk, sg=sg: e.activation(out=sg[:], in_=bk[:], func=AF.Identity,
                                                                    scale=128.0 ** -0.5),
                               reads=[bk.b], writes=[sg.b])
                    else:
                        act.op(lambda e, bk=bk, sg=sg: e.activation(out=sg[:], in_=bk[:], func=AF.Copy),
                               reads=[bk.b], writes=[sg.b])
                    if kind in ("q", "k"):
                        rb = banks[4]
                        pe.op(lambda e, sg=sg, rb=rb: e.matmul(rb[0:32, :], pm[0:32, 0:32], sg[0:32, :],
                                                               start=True, stop=True),
                              reads=[sg.b, pm.b], writes=[rb.b])
                        dve.op(lambda e, sg=sg: e.tensor_tensor(out=r2[:], in0=sg[0:32, :], in1=cos_t[:], op=ALU.mult),
                               reads=[sg.b, cos_t.b], writes=[r2.b])
                        dve.op(lambda e, rb=rb: e.tensor_tensor(out=tk[:], in0=rb[0:32, :], in1=sin_t[:], op=ALU.mult),
                               reads=[rb.b, sin_t.b], writes=[tk.b])
                        dve.op(lambda e, sg=sg: e.tensor_tensor(out=sg[0:32, :], in0=r2[:], in1=tk[:], op=ALU.add),
                               reads=[r2.b, tk.b], writes=[sg.b])
                        sp.dma(QK[di][:, tok0:tok0 + 512], sg[:], reads=[sg.b], writes=[b_QK])
                    else:
                        sp.dma(PT[di][:, tok0:tok0 + 512], sg[:], reads=[sg.b], writes=[b_PT])
        fw.barrier()

        GR = [(1, 32), (4, 8), (16, 2)]
        with ExitStack() as st:
            qT = [sbuf(st, "qT%d" % i, [128, OWN], BF16) for i in range(2)]
            kT = [sbuf(st, "kT%d" % i, [128, S_LOC], BF16) for i in range(2)]
            vh = [sbuf(st, "vh%d" % i, [128, 32, 128], BF16) for i in range(2)]
            pT = [sbuf(st, "pT%d" % i, [128, 256], BF16) for i in range(3)]
            acc = sbuf(st, "acc", [128, 2, OWN], F32)
            atb = sbuf(st, "atb", [128, OWN], BF16)
            cnt2 = {'it': 0, 'blk': 0, 'gi': 0}
            zt = sbuf(st, "zt", [128, 1024], F32)
            dve.op(lambda e: e.memset(zt[:], 0.0), writes=[zt.b])
            ztb = zt[:, :].bitcast(BF16)
            zero_jobs = []
            for e_ in range(65):
                for hf in range(2):
                    zero_jobs.append((XS[e_ * 128:(e_ + 1) * 128, hf * 2048:(hf + 1) * 2048], ztb, b_XZ))
            for hf in range(2):
                for h2 in range(2):
                    zero_jobs.append((YS[hf][64 * 128:65 * 128, h2 * 1024:(h2 + 1) * 1024], zt[:], b_YS))

            def attn_head(h):
                stages = []
                g0_last = [0]
                for g in range(3):
                    d, nb = GR[g]
                    hd = g * 8 + h
                    q_t, k_t, v_t = qT[cnt2['it'] % 2], kT[cnt2['it'] % 2], vh[cnt2['it'] % 2]
                    cnt2['it'] += 1
                    hb = nb // 2
                    lo = d * 128 * (hb - 1)
                    def loads(q_t=q_t, k_t=k_t, v_t=v_t, hd=hd, lo=lo, d=d, hb=hb, nb=nb):
                        sp.dma(q_t[:], QK[hd][:, OWN:S_LOC], writes=[q_t.b])
                        sp.dma(k_t[:, lo:S_LOC], QK[24 + hd][:, lo:S_LOC], writes=[k_t.b])
                        vsrc = VT[:, hd * 128:(hd + 1) * 128].rearrange("(n i dd) c -> dd i n c", i=128, dd=d)
                        for r in range(d):
                            sp.dma(v_t[:, r * (hb + 1):(r + 1) * (hb + 1), :], vsrc[r][:, hb - 1:nb, :],
                                   writes=[v_t.b])
                    if g < 2:
                        loads()
                    else:
                        stages[g0_last[0]] = stages[g0_last[0]][:2] + (loads,)
                    for r in range(d):
                        for n in range(hb, nb):
                            sb_, ob_ = banks[cnt2['blk'] % 2], banks[2 + cnt2['blk'] % 2]
                            p_t = pT[cnt2['blk'] % 3]
                            cnt2['blk'] += 1
                            mk = maskP if n == hb else maskR
                            qs = r + d * 128 * n - OWN
                            q_ap = q_t[:, qs:qs + d * 127 + 1:d]
                            ks0 = r + d * 128 * (n - 1)
                            ks1 = r + d * 128 * n
                            kp_ap = k_t[:, ks0:ks0 + d * 127 + 1:d]
                            kc_ap = k_t[:, ks1:ks1 + d * 127 + 1:d]
                            vi = r * (hb + 1) + (n - hb)

                            def stage_a(sb_=sb_, mk=mk, kp_ap=kp_ap, kc_ap=kc_ap, q_ap=q_ap, p_t=p_t, k_t=k_t, q_t=q_t):
                                pe.op(lambda e: e.matmul(sb_[:, 0:256], ident_b[:], mk[:], start=True, stop=False),
                                      reads=[ident_b.b, mk.b], writes=[sb_.b])
                                pe.op(lambda e: e.matmul(sb_[:, 0:128], kp_ap, q_ap, start=False, stop=False),
                                      reads=[k_t.b, q_t.b], writes=[sb_.b])
                                pe.op(lambda e: e.matmul(sb_[:, 128:256], kc_ap, q_ap, start=False, stop=True),
                                      reads=[k_t.b, q_t.b], writes=[sb_.b])
                                act.op(lambda e: e.activation(out=p_t[:], in_=sb_[:, 0:256], func=AF.Exp),
                                       reads=[sb_.b], writes=[p_t.b])

                            def stage_b(ob_=ob_, v_t=v_t, vi=vi, p_t=p_t, qs=qs, d=d, g=g):
                                pe.op(lambda e: e.matmul(ob_[:, 0:128], v_t[:, vi, :], p_t[:, 0:128], start=True, stop=False),
                                      reads=[v_t.b, p_t.b], writes=[ob_.b])
                                pe.op(lambda e: e.matmul(ob_[:, 0:128], v_t[:, vi + 1, :], p_t[:, 128:256], start=False, stop=True),
                                      reads=[v_t.b, p_t.b], writes=[ob_.b])
                                pe.op(lambda e: e.matmul(ob_[:, 128:256], ones_b[:], p_t[:, 0:128], start=True, stop=False),
                                      reads=[ones_b.b, p_t.b], writes=[ob_.b])
                                pe.op(lambda e: e.matmul(ob_[:, 128:256], ones_b[:], p_t[:, 128:256], start=False, stop=True),
                                      reads=[ones_b.b, p_t.b], writes=[ob_.b])
                                a_ap = acc[:, :, qs:qs + d * 127 + 1:d]
                                o_ap = ob_[:, 0:256].rearrange("p (a b) -> p a b", a=2)
                                if g == 0:
                                    dve.op(lambda e: e.tensor_copy(out=a_ap, in_=o_ap), reads=[ob_.b], writes=[acc.b])
                                else:
                                    dve.op(lambda e: e.tensor_tensor(out=a_ap, in0=a_ap, in1=o_ap, op=ALU.add),
                                           reads=[ob_.b, acc.b], writes=[acc.b])
                            stages.append((stage_a, stage_b, None))
                    if g == 0:
                        g0_last[0] = len(stages) - 1
                stages[0][0]()
                for i in range(len(stages)):
                    if i + 1 < len(stages):
                        stages[i + 1][0]()
                    stages[i][1]()
                    if stages[i][2] is not None:
                        stages[i][2]()
                    yield
                dve.op(lambda e: e.reciprocal(out=acc[:, 1, :], in_=acc[:, 1, :]), reads=[acc.b], writes=[acc.b])
                dve.op(lambda e: e.tensor_tensor(out=atb[:], in0=acc[:, 0, :], in1=acc[:, 1, :], op=ALU.mult),
                       reads=[acc.b], writes=[atb.b])
                pool.dma(AT[h], atb[:], reads=[atb.b], writes=[b_AT])


            cw = sbuf(st, "cw", [128, 64], F32)
            cb = sbuf(st, "cb", [128, 16], F32)
            bra = sbuf(st, "bra", [128, 16], F32)
            brx = sbuf(st, "brx", [128, 16], F32)
            lm = sbuf(st, "lm", [128, 16], F32)
            spl = sbuf(st, "spl", [128, 16], F32)
            spl2 = sbuf(st, "spl2", [128, 16], F32)
            wga = sbuf(st, "wga", [128, 2, 256], BF16)
            wgx = sbuf(st, "wgx", [128, 2, 256], BF16)
            rx = sbuf(st, "rx", [128, 2, S_LOC], BF16)
            xr = [sbuf(st, "xr%d" % i, [128, S_LOC], F32) for i in range(2)]
            xrb = sbuf(st, "xrb", [128, 2, S_LOC], BF16)
            rr = sbuf(st, "rr", [128, S_LOC], F32)
            ii = sbuf(st, "ii", [128, S_LOC], F32)
            tt = sbuf(st, "tt", [128, S_LOC], F32)
            rgb = sbuf(st, "rgb", [128, OWN], BF16)
            z1 = sbuf(st, "z1", [128, OWN], F32)
            z2 = sbuf(st, "z2", [128, OWN], F32)
            recb = sbuf(st, "recb", [128, OWN], BF16)
            sp.dma(cw[:], convw, writes=[cw.b])
            sp.dma(cb[:], convb, writes=[cb.b])
            sp.dma(bra[:], brga, writes=[bra.b])
            sp.dma(brx[:], brgx, writes=[brx.b])
            sp.dma(lm[:], lam, writes=[lm.b])
            act.op(lambda e: e.activation(out=spl[:], in_=lm[:], func=AF.Exp, scale=-1.0), reads=[lm.b], writes=[spl.b])
            act.op(lambda e: e.activation(out=spl[:], in_=spl[:], func=AF.Ln, bias=1.0), reads=[spl.b], writes=[spl.b])
            dve.op(lambda e: e.tensor_scalar(out=spl2[:], in0=spl[:], scalar1=-16.0, scalar2=None, op0=ALU.mult),
                   reads=[spl.b], writes=[spl2.b])
            dve.op(lambda e: e.tensor_scalar(out=spl[:], in0=spl[:], scalar1=-8.0, scalar2=None, op0=ALU.mult),
                   reads=[spl.b], writes=[spl.b])

            agen = [iter(())]

            def astep(n=1):
                for _ in range(n):
                    try:
                        next(agen[0])
                    except StopIteration:
                        return

            def lru_block(n):
                pool.dma(wga[:], wrga[n].rearrange("p (i j) -> p i j", i=2), writes=[wga.b])
                pool.dma(wgx[:], wrgx[n].rearrange("p (i j) -> p i j", i=2), writes=[wgx.b])
                sp.dma(rx[:], PT[2 * n:2 * n + 2].rearrange("c p t -> p c t"), writes=[rx.b])
                for ic in range(2):
                    c = 2 * n + ic
                    x_ = xr[ic]
                    cv = dve
                    cv.op(lambda e, x_=x_, ic=ic, c=c: e.tensor_scalar(out=x_[:], in0=rx[:, ic, :], scalar1=cw[:, c * 4 + 3:c * 4 + 4],
                                                                       scalar2=cb[:, c:c + 1], op0=ALU.mult, op1=ALU.add),
                           reads=[rx.b, cw.b, cb.b], writes=[x_.b])
                    for k in range(3):
                        s = 3 - k
                        cv.op(lambda e, x_=x_, ic=ic, c=c, k=k, s=s: e.scalar_tensor_tensor(
                            out=x_[:, s:S_LOC], in0=rx[:, ic, 0:S_LOC - s], scalar=cw[:, c * 4 + k:c * 4 + k + 1],
                            in1=x_[:, s:S_LOC], op0=ALU.mult, op1=ALU.add), reads=[rx.b, cw.b, x_.b], writes=[x_.b])
                        astep(1)
                    act.op(lambda e, x_=x_, ic=ic: e.activation(out=xrb[:, ic, :], in_=x_[:], func=AF.Copy),
                           reads=[x_.b], writes=[xrb.b])
                for jc in range(2):
                    c = 2 * n + jc
                    x_ = xr[jc]
                    sp.dma(rgb[:], PT[16 + c][:, OWN:S_LOC], writes=[rgb.b])
                    act.op(lambda e: e.activation(out=z1[:], in_=rgb[:], func=AF.Copy), reads=[rgb.b], writes=[z1.b])
                    pool.op(lambda e: e.tensor_tensor(out=z2[:], in0=z1[:], in1=z1[:], op=ALU.mult), reads=[z1.b], writes=[z2.b])
                    pool.op(lambda e: e.tensor_scalar(out=z2[:], in0=z2[:], scalar1=0.044715, scalar2=1.0, op0=ALU.mult, op1=ALU.add),
                            reads=[z2.b], writes=[z2.b])
                    dve.op(lambda e: e.tensor_tensor(out=z2[:], in0=z2[:], in1=z1[:], op=ALU.mult), reads=[z2.b, z1.b], writes=[z2.b])
                    act.op(lambda e: e.activation(out=z2[:], in_=z2[:], func=AF.Sigmoid, scale=1.5957691216057308),
                           reads=[z2.b], writes=[z2.b])
                    dve.op(lambda e: e.tensor_tensor(out=z2[:], in0=z2[:], in1=z1[:], op=ALU.mult), reads=[z2.b, z1.b], writes=[z2.b])
                    for (wgt, bias_t, dst) in ((wga, bra, rr), (wgx, brx, ii)):
                        for tl in range(8):
                            bk = banks[4 + cnt2['gi'] % 4]
                            cnt2['gi'] += 1
                            for ic in range(2):
                                pe.op(lambda e, bk=bk, wgt=wgt, ic=ic, jc=jc, tl=tl: e.matmul(
                                    bk[:], wgt[:, ic, jc * 128:(jc + 1) * 128], xrb[:, ic, tl * 512:(tl + 1) * 512],
                                    start=(ic == 0), stop=(ic == 1)), reads=[wgt.b, xrb.b], writes=[bk.b])
                            act.op(lambda e, bk=bk, dst=dst, bias_t=bias_t, c=c, tl=tl: e.activation(
                                out=dst[:, tl * 512:(tl + 1) * 512], in_=bk[:], func=AF.Sigmoid, bias=bias_t[:, c:c + 1]),
                                reads=[bk.b, bias_t.b], writes=[dst.b])
                            astep(1)
                    act.op(lambda e, c=c: e.activation(out=tt[:], in_=rr[:], func=AF.Exp, scale=spl2[:, c:c + 1]),
                           reads=[rr.b, spl2.b], writes=[tt.b])
                    act.op(lambda e, c=c: e.activation(out=rr[:], in_=rr[:], func=AF.Exp, scale=spl[:, c:c + 1]),
                           reads=[rr.b, spl.b], writes=[rr.b])
                    act.op(lambda e: e.activation(out=tt[:], in_=tt[:], func=AF.Sqrt, scale=-1.0, bias=1.0),
                           reads=[tt.b], writes=[tt.b])
                    dve.op(lambda e, x_=x_: e.tensor_tensor(out=ii[:], in0=ii[:], in1=x_[:], op=ALU.mult),
                           reads=[ii.b, x_.b], writes=[ii.b])
                    dve.op(lambda e: e.scalar_tensor_tensor(out=ii[:, 0:OWN], in0=ii[:, 0:OWN], scalar=flag[:, 0:1],
                                                            in1=tt[:, 0:OWN], op0=ALU.mult, op1=ALU.mult),
                           reads=[ii.b, tt.b, flag.b], writes=[ii.b])
                    dve.op(lambda e: e.tensor_tensor(out=ii[:, OWN:S_LOC], in0=ii[:, OWN:S_LOC], in1=tt[:, OWN:S_LOC], op=ALU.mult),
                           reads=[ii.b, tt.b], writes=[ii.b])
                    dve.op(lambda e: e.tensor_tensor_scan(out=tt[:], data0=rr[:], data1=ii[:], initial=0.0,
                                                          op0=ALU.mult, op1=ALU.add), reads=[rr.b, ii.b], writes=[tt.b])
                    dve.op(lambda e: e.tensor_tensor(out=recb[:], in0=z2[:], in1=tt[:, OWN:S_LOC], op=ALU.mult),
                           reads=[z2.b, tt.b], writes=[recb.b])
                    pool.dma(RT[c], recb[:], reads=[recb.b], writes=[b_RT])
            for h in range(8):
                agen[0] = attn_head(h)
                astep(2)
                lru_block(h)
                for _ in agen[0]:
                    pass
                for i in range(4 * h, 4 * h + 4):
                    pool.dma(WPB[i], wpr[i], writes=[b_AT])
                    pool.dma(WOB[i], wout[i], writes=[b_AT])
                for (zo, zi, zb) in zero_jobs[h * 17:(h + 1) * 17]:
                    sp.dma(zo, zi, reads=[zt.b], writes=[zb])
        fw.barrier()

        def layer_norm_gen(res, gt, bt, junk, st1, st2, badd=None):
            badd = badd or pool
            dve.op(lambda e: e.reduce_sum(out=st1[:, 0:1], in_=res[:], axis=AX.X), reads=[res.b], writes=[st1.b])
            dve.op(lambda e: e.tensor_scalar(out=st1[:, 1:2], in0=st1[:, 0:1], scalar1=-1.0 / D, scalar2=None, op0=ALU.mult),
                   reads=[st1.b], writes=[st1.b])
            yield
            act.op(lambda e: e.activation(out=junk[:], in_=res[:], func=AF.Square, bias=st1[:, 1:2], accum_out=st2[:, 0:1]),
                   reads=[res.b, st1.b], writes=[junk.b, st2.b])
            act.op(lambda e: e.activation(out=st2[:, 1:2], in_=st2[:, 0:1], func=AF.Sqrt, scale=1.0 / D, bias=EPS),
                   reads=[st2.b], writes=[st2.b])
            yield
            dve.op(lambda e: e.reciprocal(out=st2[:, 2:3], in_=st2[:, 1:2]), reads=[st2.b], writes=[st2.b])
            dve.op(lambda e: e.tensor_tensor(out=st2[:, 3:4], in0=st1[:, 1:2], in1=st2[:, 2:3], op=ALU.mult),
                   reads=[st1.b, st2.b], writes=[st2.b])
            act.op(lambda e: e.activation(out=res[:], in_=res[:], func=AF.Identity, scale=st2[:, 2:3], bias=st2[:, 3:4]),
                   reads=[res.b, st2.b], writes=[res.b])
            yield
            dve.op(lambda e: e.tensor_tensor(out=res[:], in0=res[:], in1=gt[:], op=ALU.mult), reads=[res.b, gt.b], writes=[res.b])
            badd.op(lambda e: e.tensor_tensor(out=res[:], in0=res[:], in1=bt[:], op=ALU.add), reads=[res.b, bt.b], writes=[res.b])
            yield

        def layer_norm(res, gt, bt, junk, st1, st2, badd=None):
            for _ in layer_norm_gen(res, gt, bt, junk, st1, st2, badd):
                pass

        with ExitStack() as st:
            ring = Ring(st, 5, "r3_")
            G1 = sbuf(st, "G1", [128, D], F32)
            B1 = sbuf(st, "B1", [128, D], F32)
            wr_s = sbuf(st, "wr_s", [128, 32, 72], F32)
            br_s = sbuf(st, "br_s", [128, 72], F32)
            att = sbuf(st, "att", [128, 8, 256], BF16)
            rect = sbuf(st, "rect", [128, 16, 256], BF16)
            mrg = sbuf(st, "mrg", [128, 32, 256], BF16)
            gts = [sbuf(st, "gts%d" % i, [128, 2, 256], BF16) for i in range(2)]
            t1 = sbuf(st, "t1", [128, 256], F32)
            t2 = sbuf(st, "t2", [128, 256], F32)
            res = [[sbuf(st, "res%d_%d" % (a, i), [128, D], F32) for i in range(2)] for a in range(2)]
            h1bs = [sbuf(st, "h1b%d" % i, [128, D], BF16) for i in range(2)]
            h1Tg = [sbuf(st, "h1Tg%d" % i, [128, 4, 128], F32) for i in range(2)]
            st1 = sbuf(st, "st1", [128, 2], F32)
            st2 = sbuf(st, "st2", [128, 4], F32)
            lgt = sbuf(st, "lgt", [128, 72], F32)
            sm = sbuf(st, "sm", [128, 16], F32)
            ohg = sbuf(st, "ohg", [128, 8], F32)
            tmp64 = sbuf(st, "tmp64", [128, 64], F32)
            les = sbuf(st, "les", [128, 8], F32)
            le2 = sbuf(st, "le2", [128, 8], F32)
            oh1 = sbuf(st, "oh1", [128, 8], F32)
            oh2 = sbuf(st, "oh2", [128, 8], F32)
            OH = [sbuf(st, "OH%d" % i, [128, 64], F32) for i in range(2)]
            Mb = sbuf(st, "Mb", [128, 64], BF16)
            runc = sbuf(st, "runc", [128, 64], F32)
            pb = sbuf(st, "pb", [128, 64], F32)
            dst_f = sbuf(st, "dst_f", [128, 4], F32)

            sp.dma(G1[:], ln1g[0:1, :].partition_broadcast(128), writes=[G1.b])
            sp.dma(B1[:], ln1b[0:1, :].partition_broadcast(128), writes=[B1.b])
            sp.dma(wr_s[:], wrt.rearrange("p (k c) -> p k c", k=32), writes=[wr_s.b])
            sp.dma(br_s[:], brt[0:1, :].partition_broadcast(128), writes=[br_s.b])
            dve.op(lambda e: e.memset(runc[:], 0.0), writes=[runc.b])

            def epi_a(rs, sub):
                h1b = h1bs[sub % 2]
                for _ in layer_norm_gen(rs, G1, B1, h1b, st1, st2, badd=dve):
                    yield
                sp.dma(H1[sub * 128:(sub + 1) * 128, :], rs[:], reads=[rs.b], writes=[b_H1])
                act.op(lambda e: e.activation(out=h1b[:], in_=rs[:], func=AF.Copy), reads=[rs.b], writes=[h1b.b])
                yield

            def epi_bc(rs, sub):
                h1b = h1bs[sub % 2]
                lb = banks[3]
                for k4 in range(8):
                    hg = h1Tg[k4 % 2]
                    tb = banks[k4 % 3]
                    for i in range(4):
                        k = k4 * 4 + i
                        pe.op(lambda e, k=k, i=i, tb=tb: e.transpose(tb[:, i * 128:(i + 1) * 128], rs[:, k * 128:(k + 1) * 128], ident_f[:]),
                              reads=[rs.b, ident_f.b], writes=[tb.b])
                    act.op(lambda e, hg=hg, tb=tb: e.activation(out=hg[:], in_=tb[:, :].rearrange("p (a b) -> p a b", a=4), func=AF.Copy),
                           reads=[tb.b], writes=[hg.b])
                    for i in range(4):
                        k = k4 * 4 + i
                        pe.op(lambda e, k=k, i=i, hg=hg: e.matmul(lb[:, 0:72], hg[:, i, :], wr_s[:, k, :], start=(k == 0), stop=(k == 31)),
                              reads=[hg.b, wr_s.b], writes=[lb.b])
                    yield
                dve.op(lambda e: e.tensor_tensor(out=lgt[:], in0=lb[:, 0:72], in1=br_s[:], op=ALU.add),
                       reads=[lb.b, br_s.b], writes=[lgt.b])
                dve.op(lambda e: e.reduce_max(out=sm[:, 0:1], in_=lgt[:, 0:8], axis=AX.X), reads=[lgt.b], writes=[sm.b])
                dve.op(lambda e: e.tensor_scalar(out=ohg[:], in0=lgt[:, 0:8], scalar1=sm[:, 0:1], scalar2=None, op0=ALU.is_equal),
                       reads=[lgt.b, sm.b], writes=[ohg.b])
                dve.op(lambda e: e.tensor_scalar(out=sm[:, 1:2], in0=sm[:, 0:1], scalar1=-1.0, scalar2=None, op0=ALU.mult),
                       reads=[sm.b], writes=[sm.b])
                yield
                act.op(lambda e: e.activation(out=les[:], in_=lgt[:, 0:8], func=AF.Exp, bias=sm[:, 1:2], accum_out=sm[:, 2:3]),
                       reads=[lgt.b, sm.b], writes=[les.b, sm.b])
                dve.op(lambda e: e.reciprocal(out=sm[:, 3:4], in_=sm[:, 2:3]), reads=[sm.b], writes=[sm.b])
                dve.op(lambda e: e.tensor_tensor(out=tmp64[:, :].rearrange("p (g j) -> p g j", g=8),
                                                 in0=lgt[:, 8:72].rearrange("p (g j) -> p g j", g=8),
                                                 in1=ohg[:, :].unsqueeze(2).broadcast_to([128, 8, 8]), op=ALU.mult),
                       reads=[lgt.b, ohg.b], writes=[tmp64.b])
                dve.op(lambda e: e.reduce_sum(out=les[:], in_=tmp64[:, :].rearrange("p (g j) -> p j g", g=8), axis=AX.X),
                       reads=[tmp64.b], writes=[les.b])
                yield
                dve.op(lambda e: e.reduce_max(out=sm[:, 4:5], in_=les[:], axis=AX.X), reads=[les.b], writes=[sm.b])
                dve.op(lambda e: e.tensor_scalar(out=oh1[:], in0=les[:], scalar1=sm[:, 4:5], scalar2=None, op0=ALU.is_equal),
                       reads=[les.b, sm.b], writes=[oh1.b])
                dve.op(lambda e: e.scalar_tensor_tensor(out=le2[:], in0=oh1[:], scalar=-1e30, in1=les[:], op0=ALU.mult, op1=ALU.add),
                       reads=[oh1.b, les.b], writes=[le2.b])
                dve.op(lambda e: e.reduce_max(out=sm[:, 5:6], in_=le2[:], axis=AX.X), reads=[le2.b], writes=[sm.b])
                yield
                dve.op(lambda e: e.tensor_scalar(out=oh2[:], in0=le2[:], scalar1=sm[:, 5:6], scalar2=None, op0=ALU.is_equal),
                       reads=[le2.b, sm.b], writes=[oh2.b])
                dve.op(lambda e: e.tensor_scalar(out=sm[:, 6:7], in0=sm[:, 4:5], scalar1=-1.0, scalar2=None, op0=ALU.mult),
                       reads=[sm.b], writes=[sm.b])
                act.op(lambda e: e.activation(out=sm[:, 7:8], in_=sm[:, 5:6], func=AF.Exp, bias=sm[:, 6:7]),
                       reads=[sm.b], writes=[sm.b])
                dve.op(lambda e: e.tensor_scalar(out=sm[:, 8:9], in0=sm[:, 7:8], scalar1=1.0, scalar2=None, op0=ALU.add),
                       reads=[sm.b], writes=[sm.b])
                yield
                dve.op(lambda e: e.reciprocal(out=sm[:, 9:10], in_=sm[:, 8:9]), reads=[sm.b], writes=[sm.b])
                dve.op(lambda e: e.tensor_tensor(out=sm[:, 10:11], in0=sm[:, 9:10], in1=sm[:, 7:8], op=ALU.mult),
                       reads=[sm.b], writes=[sm.b])
                dve.op(lambda e: e.tensor_tensor(out=wts[:, 2 * sub:2 * sub + 2], in0=sm[:, 9:11],
                                                 in1=sm[:, 3:4].broadcast_to([128, 2]), op=ALU.mult),
                       reads=[sm.b], writes=[wts.b])
                for kk, oh in enumerate((oh1, oh2)):
                    dve.op(lambda e, kk=kk, oh=oh: e.tensor_tensor(
                        out=OH[kk][:, :].rearrange("p (g j) -> p g j", g=8),
                        in0=ohg[:, :].unsqueeze(2).broadcast_to([128, 8, 8]),
                        in1=oh[:, :].unsqueeze(1).broadcast_to([128, 8, 8]), op=ALU.mult),
                        reads=[ohg.b, oh.b], writes=[OH[kk].b])
                yield
                dve.op(lambda e: e.tensor_tensor(out=Mb[:], in0=OH[0][:], in1=OH[1][:], op=ALU.add),
                       reads=[OH[0].b, OH[1].b], writes=[Mb.b])
                pe.op(lambda e: e.matmul(lb[:, 128:192], ustr[:], Mb[:], start=True, stop=True),
                      reads=[ustr.b, Mb.b], writes=[lb.b])
                pe.op(lambda e: e.matmul(lb[:, 192:256], ones_b[:], Mb[:], start=True, stop=True),
                      reads=[ones_b.b, Mb.b], writes=[lb.b])
                dve.op(lambda e: e.tensor_tensor(out=pb[:], in0=lb[:, 128:192], in1=runc[:], op=ALU.add),
                       reads=[lb.b, runc.b], writes=[pb.b])
                dve.op(lambda e: e.tensor_tensor(out=runc[:], in0=lb[:, 192:256], in1=runc[:], op=ALU.add),
                       reads=[lb.b, runc.b], writes=[runc.b])
                yield
                for kk in range(2):
                    dve.op(lambda e, kk=kk: e.tensor_tensor(out=tmp64[:], in0=OH[kk][:], in1=pb[:], op=ALU.mult),
                           reads=[OH[kk].b, pb.b], writes=[tmp64.b])
                    dve.op(lambda e: e.reduce_sum(out=dst_f[:, 0:1], in_=tmp64[:], axis=AX.X), reads=[tmp64.b], writes=[dst_f.b])
                    dve.op(lambda e, kk=kk: e.tensor_tensor(out=tmp64[:], in0=OH[kk][:], in1=e128[:], op=ALU.mult),
                           reads=[OH[kk].b, e128.b], writes=[tmp64.b])
                    dve.op(lambda e: e.reduce_sum(out=dst_f[:, 1:2], in_=tmp64[:], axis=AX.X), reads=[tmp64.b], writes=[dst_f.b])
                    yield
                    dve.op(lambda e: e.tensor_scalar(out=dst_f[:, 2:3], in0=dst_f[:, 0:1], scalar1=128.0, scalar2=None, op0=ALU.is_ge),
                           reads=[dst_f.b], writes=[dst_f.b])
                    dve.op(lambda e: e.tensor_tensor(out=dst_f[:, 1:2], in0=dst_f[:, 1:2], in1=dst_f[:, 0:1], op=ALU.add),
                           reads=[dst_f.b], writes=[dst_f.b])
                    dve.op(lambda e: e.tensor_scalar(out=dst_f[:, 3:4], in0=dst_f[:, 2:3], scalar1=-1.0, scalar2=1.0, op0=ALU.mult, op1=ALU.add),
                           reads=[dst_f.b], writes=[dst_f.b])
                    dve.op(lambda e: e.tensor_tensor(out=dst_f[:, 1:2], in0=dst_f[:, 1:2], in1=dst_f[:, 3:4], op=ALU.mult),
                           reads=[dst_f.b], writes=[dst_f.b])
                    yield
                    dve.op(lambda e: e.scalar_tensor_tensor(out=dst_f[:, 1:2], in0=dst_f[:, 2:3], scalar=dumpc[:, 0:1], in1=dst_f[:, 1:2],
                                                            op0=ALU.mult, op1=ALU.add), reads=[dst_f.b, dumpc.b], writes=[dst_f.b])
                    col = 2 * sub + kk
                    dve.op(lambda e, col=col: e.tensor_tensor(out=wts[:, col:col + 1], in0=wts[:, col:col + 1], in1=dst_f[:, 3:4], op=ALU.mult),
                           reads=[dst_f.b, wts.b], writes=[wts.b])
                    dve.op(lambda e, col=col: e.tensor_copy(out=destI[:, col:col + 1], in_=dst_f[:, 1:2]),
                           reads=[dst_f.b], writes=[destI.b])
                    pool.op(lambda e, col=col: e.indirect_dma_start(
                        out=XS[:, :], out_offset=bass.IndirectOffsetOnAxis(ap=destI[:, col:col + 1], axis=0),
                        in_=h1b[:, :], in_offset=None),
                        reads=[h1b.b, destI.b, b_XZ], writes=[b_XS], is_dma=True)
                    yield

            epi = [iter(())]
            epi_ln = [iter(())]

            def step(n=1, g=None):
                g = g or epi
                for _ in range(n):
                    try:
                        next(g[0])
                    except StopIteration:
                        return

            def drain():
                for _ in epi_ln[0]:
                    pass
                for _ in epi[0]:
                    pass

            def chain2(a, b):
                for _ in a:
                    yield
                for _ in b:
                    yield

            gcnt = 0
            for tile in range(8):
                t0 = tile * 256
                rs2 = res[tile % 2]
                sp.dma(att[:], AT[:, :, t0:t0 + 256].rearrange("c p t -> p c t"), writes=[att.b])
                sp.dma(rect[:], RT[:, :, t0:t0 + 256].rearrange("c p t -> p c t"), writes=[rect.b])
                for s_ in range(2):
                    sp.dma(rs2[s_][:], xown[t0 + s_ * 128:t0 + (s_ + 1) * 128, :], writes=[rs2[s_].b])
                for j in range(32):
                    pa = ring.load(WPB[j], 3072)
                    pav = pa[:, 0:1024].rearrange("p (k c) -> p k c", k=8)
                    prv = pa[:, 1024:3072].rearrange("p (k c) -> p k c", k=16)
                    yb_ = banks[gcnt % 2]
                    gt = gts[gcnt % 2]
                    gcnt += 1
                    pool.dma(gt[:], PT[32 + j:96:32][:, :, OWN + t0:OWN + t0 + 256].rearrange("g p t -> p g t"),
                             writes=[gt.b])
                    for k in range(8):
                        pe.op(lambda e, yb_=yb_, pav=pav, k=k: e.matmul(yb_[:, 0:256], pav[:, k, :], att[:, k, :], start=(k == 0), stop=(k == 7)),
                              reads=[pa.b, att.b], writes=[yb_.b])
                    for k in range(16):
                        pe.op(lambda e, yb_=yb_, prv=prv, k=k: e.matmul(yb_[:, 256:512], prv[:, k, :], rect[:, k, :], start=(k == 0), stop=(k == 15)),
                              reads=[pa.b, rect.b], writes=[yb_.b])
                    dve.op(lambda e, gt=gt, yb_=yb_: e.tensor_tensor(out=t1[:], in0=gt[:, 0, :], in1=yb_[:, 0:256], op=ALU.mult),
                           reads=[gt.b, yb_.b], writes=[t1.b])
                    dve.op(lambda e, gt=gt, yb_=yb_: e.tensor_tensor(out=t2[:], in0=gt[:, 1, :], in1=yb_[:, 256:512], op=ALU.mult),
                           reads=[gt.b, yb_.b], writes=[t2.b])
                    dve.op(lambda e, j=j: e.tensor_tensor(out=mrg[:, j, :], in0=t1[:], in1=t2[:], op=ALU.add),
                           reads=[t1.b, t2.b], writes=[mrg.b])
                    if j % 3 == 2:
                        step(1, epi_ln)
                for _ in epi_ln[0]:
                    pass
                for eg in range(8):
                    for q in range(4):
                        po = ring.load(WOB[eg * 4 + q])
                        pov = po[:, :].rearrange("p (k e) -> p k e", k=8)
                        for s_ in range(2):
                            bk = banks[4 + 2 * (eg % 2) + s_]
                            for kk in range(8):
                                pe.op(lambda e, bk=bk, pov=pov, q=q, kk=kk, s_=s_: e.matmul(
                                    bk[:], mrg[:, q * 8 + kk, s_ * 128:(s_ + 1) * 128], pov[:, kk, :],
                                    start=(q == 0 and kk == 0), stop=(q == 3 and kk == 7)),
                                    reads=[po.b, mrg.b], writes=[bk.b])
                        step(2 if (eg * 4 + q) % 4 == 0 else 1)
                    for s_ in range(2):
                        bk = banks[4 + 2 * (eg % 2) + s_]
                        rs = rs2[s_]
                        dve.op(lambda e, bk=bk, rs=rs, eg=eg: e.scalar_tensor_tensor(
                            out=rs[:, eg * 512:(eg + 1) * 512], in0=rs[:, eg * 512:(eg + 1) * 512], scalar=ALPHA,
                            in1=bk[:], op0=ALU.mult, op1=ALU.add), reads=[bk.b, rs.b], writes=[rs.b])
                drain()
                epi_ln[0] = chain2(epi_a(rs2[0], tile * 2), epi_a(rs2[1], tile * 2 + 1))
                epi[0] = chain2(epi_bc(rs2[0], tile * 2), epi_bc(rs2[1], tile * 2 + 1))
            drain()
        fw.barrier()

        with ExitStack() as st:
            ring = Ring(st, 14, "r4_")
            xe = [sbuf(st, "xe%d" % i, [128, D], BF16) for i in range(2)]
            xeT = [sbuf(st, "xeT%d" % i, [128, 32, 128], BF16) for i in range(2)]
            sg_t = sbuf(st, "sg_t", [128, 512], F32)
            hid = [sbuf(st, "hid%d" % i, [128, 512], BF16) for i in range(2)]
            ye = [sbuf(st, "ye%d" % i, [128, D], F32) for i in range(2)]
            ev = 0
            for e_ in range(64):
                x_e, x_T, hd_, y_e = xe[e_ % 2], xeT[e_ % 2], hid[e_ % 2], ye[e_ % 2]
                if e_ == 0:
                    sp.dma(x_e[:], XS[0:128, :], writes=[x_e.b])
                for k8 in range(4):
                    bk = banks[k8 % 2]
                    bkb = bk[:, :].bitcast(BF16)
                    for i in range(8):
                        k = k8 * 8 + i
                        pe.op(lambda e, bkb=bkb, x_e=x_e, k=k, i=i: e.transpose(bkb[:, i * 128:(i + 1) * 128], x_e[:, k * 128:(k + 1) * 128], ident_b[:]),
                              reads=[x_e.b, ident_b.b], writes=[bk.b])
                    act.op(lambda e, bkb=bkb, x_T=x_T, k8=k8: e.activation(
                        out=x_T[:, k8 * 8:(k8 + 1) * 8, :], in_=bkb.rearrange("p (a b) -> p a b", a=8), func=AF.Copy),
                        reads=[bk.b], writes=[x_T.b])
                gb, ub = banks[2], banks[3]
                for q in range(4):
                    pg = ring.load(wg[e_][q])
                    pu = ring.load(wu[e_][q])
                    for (pp, bk) in ((pg, gb), (pu, ub)):
                        ppv = pp[:, :].rearrange("p (k f) -> p k f", k=8)
                        for fc in range(4):
                            for kk in range(8):
                                pe.op(lambda e, bk=bk, ppv=ppv, fc=fc, kk=kk, q=q, x_T=x_T: e.matmul(
                                    bk[:, fc * 128:(fc + 1) * 128], ppv[:, kk, fc * 128:(fc + 1) * 128], x_T[:, q * 8 + kk, :],
                                    start=(q == 0 and kk == 0 and fc == 0), stop=(q == 3 and kk == 7 and fc == 3),
                                    skip_group_check=True),
                                    reads=[pp.b, x_T.b], writes=[bk.b])
                act.op(lambda e, gb=gb: e.activation(out=sg_t[:], in_=gb[:], func=AF.Silu), reads=[gb.b], writes=[sg_t.b])
                dve.op(lambda e, ub=ub, hd_=hd_: e.tensor_tensor(out=hd_[:], in0=sg_t[:], in1=ub[:], op=ALU.mult),
                       reads=[sg_t.b, ub.b], writes=[hd_.b])
                pds = [ring.load(wd[e_][fc * 128:(fc + 1) * 128, :]) for fc in range(4)]
                for dg in range(8):
                    bk = banks[4 + dg % 4]
                    for fc in range(4):
                        pe.op(lambda e, bk=bk, fc=fc, dg=dg, hd_=hd_, pd=pds[fc]: e.matmul(
                            bk[:], hd_[:, fc * 128:(fc + 1) * 128], pd[:, dg * 512:(dg + 1) * 512], start=(fc == 0), stop=(fc == 3)),
                            reads=[hd_.b, pds[fc].b], writes=[bk.b])
                    if ev % 2 == 0:
                        act.op(lambda e, bk=bk, y_e=y_e, dg=dg: e.activation(out=y_e[:, dg * 512:(dg + 1) * 512], in_=bk[:], func=AF.Copy),
                               reads=[bk.b], writes=[y_e.b])
                    else:
                        dve.op(lambda e, bk=bk, y_e=y_e, dg=dg: e.tensor_copy(out=y_e[:, dg * 512:(dg + 1) * 512], in_=bk[:]),
                               reads=[bk.b], writes=[y_e.b])
                    ev += 1
                if e_ + 1 < 64:
                    xn = xe[(e_ + 1) % 2]
                    sp.dma(xn[:], XS[(e_ + 1) * 128:(e_ + 2) * 128, :], writes=[xn.b])
                for hf in range(2):
                    sp.dma(YS[hf][e_ * 128:(e_ + 1) * 128, :], y_e[:, hf * 2048:(hf + 1) * 2048], reads=[y_e.b], writes=[b_YS])
        fw.barrier()

        with ExitStack() as st:
            G2 = sbuf(st, "G2", [128, D], F32)
            B2 = sbuf(st, "B2", [128, D], F32)
            hr = [sbuf(st, "hr%d" % i, [128, D], F32) for i in range(2)]
            ya = [sbuf(st, "ya%d" % i, [128, D], F32) for i in range(2)]
            yb = [sbuf(st, "yb%d" % i, [128, D], F32) for i in range(2)]
            junk = sbuf(st, "junk", [128, D], BF16)
            st1 = sbuf(st, "st1b", [128, 2], F32)
            st2 = sbuf(st, "st2b", [128, 4], F32)
            sp.dma(G2[:], ln2g[0:1, :].partition_broadcast(128), writes=[G2.b])
            sp.dma(B2[:], ln2b[0:1, :].partition_broadcast(128), writes=[B2.b])
            for i in range(2):
                dve.op(lambda e, i=i: e.memset(ya[i][:], 0.0), writes=[ya[i].b])
                dve.op(lambda e, i=i: e.memset(yb[i][:], 0.0), writes=[yb[i].b])
            def fetch(sub):
                h_, a_, b_ = hr[sub % 2], ya[sub % 2], yb[sub % 2]
                sp.dma(h_[:], H1[sub * 128:(sub + 1) * 128, :], writes=[h_.b])
                for (dstt, col) in ((a_, 2 * sub), (b_, 2 * sub + 1)):
                    for hf in range(2):
                        pool.op(lambda e, dstt=dstt, col=col, hf=hf: e.indirect_dma_start(
                            out=dstt[:, hf * 2048:(hf + 1) * 2048], out_offset=None, in_=YS[hf][:, :],
                            in_offset=bass.IndirectOffsetOnAxis(ap=destI[:, col:col + 1], axis=0)),
                            reads=[destI.b], writes=[dstt.b], is_dma=True)

            fetch(0)
            for sub in range(16):
                h_, a_, b_ = hr[sub % 2], ya[sub % 2], yb[sub % 2]
                act.op(lambda e, h_=h_: e.activation(out=h_[:], in_=h_[:], func=AF.Identity, scale=ALPHA), reads=[h_.b], writes=[h_.b])
                dve.op(lambda e, h_=h_, a_=a_, sub=sub: e.scalar_tensor_tensor(out=h_[:], in0=a_[:], scalar=wts[:, 2 * sub:2 * sub + 1],
                                                                              in1=h_[:], op0=ALU.mult, op1=ALU.add),
                       reads=[a_.b, h_.b, wts.b], writes=[h_.b])
                dve.op(lambda e, h_=h_, b_=b_, sub=sub: e.scalar_tensor_tensor(out=h_[:], in0=b_[:], scalar=wts[:, 2 * sub + 1:2 * sub + 2],
                                                                              in1=h_[:], op0=ALU.mult, op1=ALU.add),
                       reads=[b_.b, h_.b, wts.b], writes=[h_.b])
                if sub + 1 < 16:
                    fetch(sub + 1)
                layer_norm(h_, G2, B2, junk, st1, st2, badd=dve)
                pool.dma(out[sub * 128:(sub + 1) * 128, :], h_[:], reads=[h_.b], writes=[b_out])

        fw.emit(top)
    return nc


def _consts(half):
    ident = np.eye(128, dtype=np.float32)
    NEG = -30000.0
    k = np.arange(128)[:, None]
    q = np.arange(128)[None, :]
    mprev = np.where(k >= q, 0.0, NEG).astype(np.float32)
    mcur = np.where(k <= q, 0.0, NEG).astype(np.float32)
    maskR = np.concatenate([mprev, mcur], axis=1)
    maskP = maskR.copy()
    if half == 0:
        maskP[:, :128] = NEG
    pm = np.zeros((32, 32), np.float32)
    for m in range(16):
        pm[m + 16, m] = -1.0
    for m in range(16, 32):
        pm[m - 16, m] = 1.0
    invf = np.power(np.float32(500000.0), -np.arange(16, dtype=np.float32) * np.float32(2.0) / np.float32(32)).astype(np.float32)
    invf = np.concatenate([invf, invf]).reshape(32, 1)
    flag = np.full((128, 1), float(half), np.float32)
    ustr = (np.arange(128)[:, None] < np.arange(128)[None, :]).astype(np.float32)
    e128 = np.tile((np.arange(64, dtype=np.float32) * 128.0)[None, :], (128, 1))
    dump = (8192.0 + np.arange(128, dtype=np.float32)).reshape(128, 1)
    return dict(c_dump=dump, c_ident=ident, c_maskR=maskR, c_maskP=maskP, c_pm=pm, c_invf=invf, c_flag=flag, c_ustr=ustr, c_e128=e128)


def _prep_shared(inp):
    f = np.float32
    sh = {}
    w_in = np.asarray(inp["w_in"], f)[0]
    sh["win"] = np.ascontiguousarray(w_in.reshape(32, 128, NCH_IN, 128).transpose(2, 1, 0, 3)).reshape(NCH_IN, 128, 4096)
    bgt = np.asarray(inp["b_gate"], f)[0]
    sh["bgate"] = np.ascontiguousarray(bgt.reshape(2, 32, 128).transpose(2, 0, 1)).reshape(128, 64)
    cw = np.asarray(inp["conv_w"], f)[0]
    sh["convw"] = np.ascontiguousarray(cw.reshape(4, 16, 128).transpose(2, 1, 0)).reshape(128, 64)
    sh["convb"] = np.ascontiguousarray(np.asarray(inp["conv_b"], f)[0].reshape(16, 128).T)
    for nm, key in (("wrga", "w_rg_a"), ("wrgx", "w_rg_x")):
        w = np.asarray(inp[key], f)[0]
        sh[nm] = np.ascontiguousarray(w.reshape(8, 2, 128, 256).transpose(0, 2, 1, 3)).reshape(8, 128, 512)
    for nm, key in (("brga", "b_rg_a"), ("brgx", "b_rg_x")):
        sh[nm] = np.ascontiguousarray(np.asarray(inp[key], f)[0].reshape(16, 128).T)
    sh["lam"] = np.ascontiguousarray(np.asarray(inp["lru_lambda"], f)[0].reshape(16, 128).T)
    wa = np.asarray(inp["w_attn_proj"], f)[0]
    wa_l = wa.reshape(8, 128, 32, 128).transpose(2, 1, 0, 3).reshape(32, 128, 1024)
    wr = np.asarray(inp["w_rec_proj"], f)[0]
    wr_l = wr.reshape(16, 128, 32, 128).transpose(2, 1, 0, 3).reshape(32, 128, 2048)
    sh["wpr"] = np.ascontiguousarray(np.concatenate([wa_l, wr_l], axis=2))
    wo = np.asarray(inp["w_out"], f)[0]
    sh["wout"] = np.ascontiguousarray(wo.reshape(4, 8, 128, 8, 512).transpose(3, 0, 2, 1, 4)).reshape(32, 128, 4096)
    sh["ln1g"] = np.asarray(inp["ln1_g"], f).reshape(1, D)
    sh["ln1b"] = np.asarray(inp["ln1_b"], f).reshape(1, D)
    wrt = np.concatenate([np.asarray(inp["w_router_group"], f)[0], np.asarray(inp["w_router_expert"], f)[0]], axis=1)
    sh["wrt"] = np.ascontiguousarray(wrt.reshape(32, 128, 72).transpose(1, 0, 2)).reshape(128, 32 * 72)
    sh["brt"] = np.concatenate([np.asarray(inp["b_router_group"], f)[0], np.asarray(inp["b_router_expert"], f)[0]]).reshape(1, 72)
    for nm, key in (("wg", "w_gate"), ("wu", "w_up")):
        w = np.asarray(inp[key], f)[0]
        sh[nm] = np.ascontiguousarray(w.reshape(64, 4, 8, 128, 512).transpose(0, 1, 3, 2, 4)).reshape(64, 4, 128, 4096)
    sh["wd"] = np.asarray(inp["w_down"], f)[0]
    sh["ln2g"] = np.asarray(inp["ln2_g"], f).reshape(1, D)
    sh["ln2b"] = np.asarray(inp["ln2_b"], f).reshape(1, D)
    return sh


def kernel(**inp):
    x = np.asarray(inp["x"], np.float32)
    positions = np.asarray(inp["positions"], np.int32)
    sh = _prep_shared(inp)
    in_maps = []
    for c in range(8):
        b, half = c // 2, c % 2
        xl = np.zeros((S_LOC, D), np.float32)
        pl = np.zeros((1, S_LOC), np.int32)
        if half == 1:
            xl[:] = x[b]
            pl[0] = positions[b]
        else:
            xl[OWN:] = x[b, :OWN]
            pl[0, OWN:] = positions[b, :OWN]
        xT = np.ascontiguousarray(xl.reshape(8, 512, 32, 128).transpose(0, 3, 2, 1)).reshape(8, 128, 32 * 512)
        m = dict(sh)
        m["xT"] = xT
        m["xown"] = np.ascontiguousarray(x[b, half * OWN:(half + 1) * OWN])
        m["pos"] = pl
        m.update(_consts(half))
        in_maps.append(m)
    nc = build_nc()
    res = run_bass_kernel_spmd(nc, in_maps, core_ids=list(range(8)))
    outp = np.zeros((4, 4096, D), np.float32)
    for c in range(8):
        b, half = c // 2, c % 2
        outp[b, half * OWN:(half + 1) * OWN] = np.asarray(res.results[c]["out"], np.float32)
    return outp
```

```python
import math
from contextlib import ExitStack
import numpy as np
import concourse.bass as bass
import concourse.mybir as mybir
from concourse.bass_utils import run_bass_kernel_spmd

F32 = mybir.dt.float32
BF16 = mybir.dt.bfloat16
I32 = mybir.dt.int32
AF = mybir.ActivationFunctionType
ALU = mybir.AluOpType
AX = mybir.AxisListType

SAME_ENGINE_SYNC = True
N_DMA_SEMS = 24

S_LOC = 4096
OWN = 2048
D = 4096
NCH_IN = 168
ALPHA = 2.0 ** 0.25
EPS = 1e-5
MAGIC = 12582912.0
TWO_PI = 2.0 * math.pi


class Buf:
    __slots__ = ("name", "w", "r", "multi", "ws")

    def __init__(self, name="", multi=False):
        self.name = name
        self.w = None
        self.r = {}
        self.multi = multi
        self.ws = []


class Op:
    __slots__ = ("eng", "fn", "deps", "sig", "sigval", "is_dma", "dsem", "dval")

    def __init__(self, eng, fn, is_dma):
        self.eng = eng
        self.fn = fn
        self.deps = []
        self.sig = False
        self.sigval = None
        self.is_dma = is_dma
        self.dsem = None
        self.dval = None


class Eng:
    def __init__(self, fw, name):
        self.fw = fw
        self.name = name
        self.ops = []
        self.sem = None
        self.dsems = []
        self.pending = []

    def op(self, fn, reads=(), writes=(), is_dma=False):
        o = Op(self, fn, is_dma)
        deps = list(self.pending)
        self.pending = []
        for b in reads:
            if b.multi:
                deps.extend(b.ws)
            elif b.w is not None:
                deps.append(b.w)
        for b in writes:
            if b.multi:
                continue
            if b.w is not None:
                deps.append(b.w)
            deps.extend(b.r.values())
        seen = set()
        for d in deps:
            if d is o or id(d) in seen:
                continue
            seen.add(id(d))
            if (not d.is_dma) and d.eng is self and not is_dma:
                if self.name == "pe" or not SAME_ENGINE_SYNC:
                    continue
            d.sig = True
            o.deps.append(d)
        key = (self.name, is_dma)
        for b in reads:
            if b.multi:
                continue
            if is_dma:
                b.r[(key, len(b.r))] = o
            else:
                b.r[key] = o
        for b in writes:
            if b.multi:
                b.ws.append(o)
                continue
            b.w = o
            b.r = {}
        self.ops.append(o)
        self.fw.all_ops.append(o)
        return o

    def dma(self, out, in_, reads=(), writes=(), **kw):
        return self.op(lambda e: e.dma_start(out=out, in_=in_, **kw), reads, writes, is_dma=True)


class FW:
    def __init__(self, nc):
        self.nc = nc
        self.all_ops = []
        self.pe = Eng(self, "pe")
        self.act = Eng(self, "act")
        self.dve = Eng(self, "dve")
        self.pool = Eng(self, "pool")
        self.sp = Eng(self, "sp")
        self.engs = [self.pe, self.act, self.dve, self.pool, self.sp]
        self.mark = 0

    def barrier(self):
        lasts = []
        for e in self.engs:
            last_c = None
            for o in reversed(e.ops):
                if not o.is_dma:
                    last_c = o
                    break
            if last_c is not None:
                lasts.append(last_c)
        dm = []
        for e in self.engs:
            dl = [o for o in e.ops if o.is_dma]
            dm.extend(dl[-N_DMA_SEMS:])
        for e in self.engs:
            e.pending = list(e.pending) + lasts + dm

    def emit(self, stack):
        nc = self.nc
        for e in self.engs:
            e.sem = stack.enter_context(nc.semaphore("s_" + e.name))
            if e.name in ("sp", "act", "pool"):
                e.dsems = [stack.enter_context(nc.semaphore("d_%s_%d" % (e.name, i))) for i in range(N_DMA_SEMS)]
        for e in self.engs:
            c = 0
            k = 0
            uses = [0] * max(1, len(e.dsems))
            for o in e.ops:
                if o.is_dma:
                    s = k % len(e.dsems)
                    k += 1
                    uses[s] += 1
                    o.dsem = e.dsems[s]
                    o.dval = 16 * uses[s]
                elif o.sig:
                    c += 1
                    o.sigval = c
        block = stack.enter_context(nc.Block())

        def run(eng_obj, h):
            seen = {}

            def wait(sem, val):
                key = id(sem)
                if seen.get(key, 0) >= val:
                    return
                seen[key] = val
                h.wait_ge(sem, val)

            for o in eng_obj.ops:
                for d in o.deps:
                    if d.is_dma:
                        wait(d.dsem, d.dval)
                    else:
                        wait(d.eng.sem, d.sigval)
                if o.is_dma:
                    if o.dval > 16:
                        wait(o.dsem, o.dval - 16)
                    o.fn(h).then_inc(o.dsem, 16)
                else:
                    ins = o.fn(h)
                    if o.sig:
                        ins.then_inc(eng_obj.sem, 1)
            last = {}
            for o in eng_obj.ops:
                if o.is_dma:
                    last[id(o.dsem)] = (o.dsem, o.dval)
            for sem, val in last.values():
                wait(sem, val)

        @block.tensor
        def _(h):
            run(self.pe, h)

        @block.scalar
        def _(h):
            run(self.act, h)

        @block.vector
        def _(h):
            run(self.dve, h)

        @block.gpsimd
        def _(h):
            run(self.pool, h)

        @block.sync
        def _(h):
            run(self.sp, h)


class T:
    def __init__(self, h, name):
        self.h = h
        self.b = Buf(name)

    def __getitem__(self, k):
        return self.h[k]


def build_nc():
    nc = bass.Bass("TRN2", target_bir_lowering=False)

    def din(name, shape, dt=F32):
        return nc.dram_tensor(name, list(shape), dt, kind="ExternalInput").ap()

    def dscr(name, shape, dt):
        return nc.dram_tensor(name, list(shape), dt, kind="Internal").ap()

    xT = din("xT", [8, 128, 32 * 512])
    xown = din("xown", [OWN, D])
    pos = din("pos", [1, S_LOC], I32)
    win = din("win", [NCH_IN, 128, 4096])
    bgate = din("bgate", [128, 64])
    convw = din("convw", [128, 16 * 4])
    convb = din("convb", [128, 16])
    wrga = din("wrga", [8, 128, 512])
    wrgx = din("wrgx", [8, 128, 512])
    brga = din("brga", [128, 16])
    brgx = din("brgx", [128, 16])
    lam = din("lam", [128, 16])
    wpr = din("wpr", [32, 128, 3072])
    wout = din("wout", [32, 128, 4096])
    ln1g = din("ln1g", [1, D])
    ln1b = din("ln1b", [1, D])
    wrt = din("wrt", [128, 32 * 72])
    brt = din("brt", [1, 72])
    wg = din("wg", [64, 4, 128, 4096])
    wu = din("wu", [64, 4, 128, 4096])
    wd = din("wd", [64, 512, D])
    ln2g = din("ln2g", [1, D])
    ln2b = din("ln2b", [1, D])
    c_ident = din("c_ident", [128, 128])
    c_maskR = din("c_maskR", [128, 256])
    c_maskP = din("c_maskP", [128, 256])
    c_pm = din("c_pm", [32, 32])
    c_invf = din("c_invf", [32, 1])
    c_flag = din("c_flag", [128, 1])
    c_ustr = din("c_ustr", [128, 128])
    c_e128 = din("c_e128", [128, 64])
    c_dump = din("c_dump", [128, 1])
    out = nc.dram_tensor("out", [OWN, D], F32, kind="ExternalOutput").ap()

    QK = dscr("QK", [48, 128, S_LOC], BF16)
    VT = dscr("VT", [S_LOC, 3072], BF16)
    PT = dscr("PT", [96, 128, S_LOC], BF16)
    AT = dscr("AT", [8, 128, OWN], BF16)
    RT = dscr("RT", [16, 128, OWN], BF16)
    H1 = dscr("H1", [OWN, D], F32)
    WPB = dscr("WPB", [32, 128, 3072], BF16)
    WOB = dscr("WOB", [32, 128, 4096], BF16)
    XS = dscr("XS", [65 * 128, D], BF16)
    YS = [dscr("YS%d" % i, [65 * 128, 2048], F32) for i in range(2)]

    fw = FW(nc)
    pe, act, dve, pool, sp = fw.pe, fw.act, fw.dve, fw.pool, fw.sp
    b_QK, b_VT, b_PT, b_AT, b_RT, b_H1, b_XS, b_YS, b_out = [Buf(n, multi=True) for n in "QK VT PT AT RT H1 XS YS out".split()]
    b_XZ = Buf("XSzero", multi=True)

    with ExitStack() as top:
        def sbuf(st, name, shape, dt):
            return T(st.enter_context(nc.sbuf_tensor(name, list(shape), dt)), name)

        banks = [T(top.enter_context(nc.psum_tensor("bank%d" % i, [128, 512], F32)), "bank%d" % i) for i in range(8)]

        ident_f = sbuf(top, "ident_f", [128, 128], F32)
        ident_b = sbuf(top, "ident_b", [128, 128], BF16)
        ones_b = sbuf(top, "ones_b", [128, 128], BF16)
        maskR = sbuf(top, "maskR", [128, 256], BF16)
        maskP = sbuf(top, "maskP", [128, 256], BF16)
        pm = sbuf(top, "pm", [32, 32], BF16)
        invf = sbuf(top, "invf", [32, 1], F32)
        flag = sbuf(top, "flag", [128, 1], F32)
        ustr = sbuf(top, "ustr", [128, 128], BF16)
        e128 = sbuf(top, "e128", [128, 64], F32)
        dumpc = sbuf(top, "dumpc", [128, 1], F32)
        bg = sbuf(top, "bg", [128, 64], F32)
        destI = sbuf(top, "destI", [128, 32], I32)
        wts = sbuf(top, "wts", [128, 32], F32)

        sp.dma(ident_f[:], c_ident, writes=[ident_f.b])
        pool.dma(ident_b[:], c_ident, writes=[ident_b.b])
        pool.dma(maskR[:], c_maskR, writes=[maskR.b])
        pool.dma(maskP[:], c_maskP, writes=[maskP.b])
        pool.dma(pm[:], c_pm, writes=[pm.b])
        pool.dma(ustr[:], c_ustr, writes=[ustr.b])
        sp.dma(invf[:], c_invf, writes=[invf.b])
        sp.dma(flag[:], c_flag, writes=[flag.b])
        sp.dma(e128[:], c_e128, writes=[e128.b])
        sp.dma(dumpc[:], c_dump, writes=[dumpc.b])
        sp.dma(bg[:], bgate, writes=[bg.b])
        dve.op(lambda e: e.memset(ones_b[:], 1.0), writes=[ones_b.b])

        class Ring:
            def __init__(self, st, n, name):
                self.slots = [sbuf(st, "%s%d" % (name, i), [128, 4096], BF16) for i in range(n)]
                self.i = 0

            def load(self, src, nel=4096, k=None):
                s = self.slots[self.i % len(self.slots)]
                self.i += 1
                o = s[:, 0:nel]
                if k is not None:
                    o = o.rearrange("p (k e) -> p k e", k=k)
                pool.dma(o, src, writes=[s.b])
                return s

        with ExitStack() as st:
            ring = Ring(st, 8, "r1_")
            xt = [sbuf(st, "xt%d" % i, [128, 32, 512], BF16) for i in range(2)]
            stg = [sbuf(st, "stg%d" % i, [128, 512], BF16) for i in range(4)]
            posi = sbuf(st, "posi", [32, 512], I32)
            ang = sbuf(st, "ang", [32, 512], F32)
            tk = sbuf(st, "tk", [32, 512], F32)
            cos_t = sbuf(st, "cos_t", [32, 512], F32)
            sin_t = sbuf(st, "sin_t", [32, 512], F32)
            r1 = sbuf(st, "rp1", [32, 512], F32)
            r2 = sbuf(st, "rp2", [32, 512], F32)

            def load_xt(lt):
                t = xt[lt % 2]
                for q in range(4):
                    pool.dma(t[:, q * 8:(q + 1) * 8, :],
                             xT[lt][:, q * 4096:(q + 1) * 4096].rearrange("p (k t) -> p k t", k=8),
                             writes=[t.b])

            def trig(dst, shift):
                dve.op(lambda e: e.tensor_scalar(out=r1[:], in0=ang[:], scalar1=shift, scalar2=None, op0=ALU.add),
                       reads=[ang.b], writes=[r1.b])
                dve.op(lambda e: e.tensor_scalar(out=tk[:], in0=r1[:], scalar1=1.0 / TWO_PI, scalar2=MAGIC,
                                                 op0=ALU.mult, op1=ALU.add), reads=[r1.b], writes=[tk.b])
                dve.op(lambda e: e.tensor_scalar(out=tk[:], in0=tk[:], scalar1=-MAGIC, scalar2=None, op0=ALU.add),
                       reads=[tk.b], writes=[tk.b])
                dve.op(lambda e: e.scalar_tensor_tensor(out=r1[:], in0=tk[:], scalar=-TWO_PI, in1=r1[:],
                                                        op0=ALU.mult, op1=ALU.add), reads=[tk.b, r1.b], writes=[r1.b])
                dve.op(lambda e: e.tensor_scalar(out=r1[:], in0=r1[:], scalar1=3.14159, scalar2=-3.14159,
                                                 op0=ALU.min, op1=ALU.max), reads=[r1.b], writes=[r1.b])
                act.op(lambda e: e.activation(out=dst[:], in_=r1[:], func=AF.Sin), reads=[r1.b], writes=[dst.b])

            def chunks_for(lt):
                L = []
                own = lt >= 4
                for hd in range(24):
                    g = hd // 8
                    need_kv = own or g == 2 or lt == 3
                    if own:
                        L.append((hd, "q", hd))
                    if need_kv:
                        L.append((24 + hd, "k", 24 + hd))
                        L.append((48 + hd, "v", hd))
                for c in range(16):
                    L.append((72 + c, "p", c))
                if own:
                    for c in range(16):
                        L.append((88 + c, "p", 16 + c))
                    for c in range(64):
                        L.append((104 + c, "g", 32 + c))
                return L

            load_xt(0)
            cnt = 0
            for lt in range(8):
                x_t = xt[lt % 2]
                tok0 = lt * 512
                if lt + 1 < 8:
                    load_xt(lt + 1)
                sp.dma(posi[:], pos[0:1, tok0:tok0 + 512].partition_broadcast(32), writes=[posi.b])
                dve.op(lambda e: e.tensor_copy(out=ang[:], in_=posi[:]), reads=[posi.b], writes=[ang.b])
                dve.op(lambda e: e.tensor_scalar(out=ang[:], in0=ang[:], scalar1=invf[:, 0:1], scalar2=None, op0=ALU.mult),
                       reads=[ang.b, invf.b], writes=[ang.b])
                trig(sin_t, 0.0)
                trig(cos_t, math.pi / 2)
                for (wc, kind, di) in chunks_for(lt):
                    wt = ring.load(win[wc])
                    wv = wt[:, :].rearrange("p (k c) -> p k c", k=32)
                    bk = banks[cnt % 4]
                    sg = stg[cnt % 4]
                    cnt += 1
                    if kind == "v":
                        for s in range(4):
                            for k in range(32):
                                pe.op(lambda e, s=s, k=k, bk=bk, wv=wv, x_t=x_t: e.matmul(
                                    bk[:, s * 128:(s + 1) * 128], x_t[:, k, s * 128:(s + 1) * 128], wv[:, k, :],
                                    start=(k == 0), stop=(k == 31)),
                                    reads=[x_t.b, wt.b], writes=[bk.b])
                        act.op(lambda e, bk=bk, sg=sg: e.activation(out=sg[:], in_=bk[:], func=AF.Copy),
                               reads=[bk.b], writes=[sg.b])
                        sp.dma(VT[tok0:tok0 + 512, di * 128:(di + 1) * 128].rearrange("(s p) c -> p s c", p=128),
                               sg[:, :].rearrange("p (s c) -> p s c", s=4), reads=[sg.b], writes=[b_VT])
                        continue
                    for k in range(32):
                        pe.op(lambda e, k=k, bk=bk, wv=wv, x_t=x_t: e.matmul(
                            bk[:], wv[:, k, :], x_t[:, k, :], start=(k == 0), stop=(k == 31)),
                            reads=[x_t.b, wt.b], writes=[bk.b])
                    if kind == "g":
                        col = di - 32
                        act.op(lambda e, bk=bk, sg=sg, col=col: e.activation(out=sg[:], in_=bk[:], func=AF.Sigmoid,
                                                                             bias=bg[:, col:col + 1]),
                               reads=[bk.b, bg.b], writes=[sg.b])
                    elif kind == "q":
                        act.op(lambda e, bk=bk, sg=sg: e.activation(out=sg[:], in_=bk[:], func=AF.Identity,
                                                                    scale=128.0 ** -0.5),
                               reads=[bk.b], writes=[sg.b])
                    else:
                        act.op(lambda e, bk=bk, sg=sg: e.activation(out=sg[:], in_=bk[:], func=AF.Copy),
                               reads=[bk.b], writes=[sg.b])
                    if kind in ("q", "k"):
                        rb = banks[4]
                        pe.op(lambda e, sg=sg, rb=rb: e.matmul(rb[0:32, :], pm[0:32, 0:32], sg[0:32, :],
                                                               start=True, stop=True),
                              reads=[sg.b, pm.b], writes=[rb.b])
                        dve.op(lambda e, sg=sg: e.tensor_tensor(out=r2[:], in0=sg[0:32, :], in1=cos_t[:], op=ALU.mult),
                               reads=[sg.b, cos_t.b], writes=[r2.b])
                        dve.op(lambda e, rb=rb: e.tensor_tensor(out=tk[:], in0=rb[0:32, :], in1=sin_t[:], op=ALU.mult),
                               reads=[rb.b, sin_t.b], writes=[tk.b])
                        dve.op(lambda e, sg=sg: e.tensor_tensor(out=sg[0:32, :], in0=r2[:], in1=tk[:], op=ALU.add),
                               reads=[r2.b, tk.b], writes=[sg.b])
                        sp.dma(QK[di][:, tok0:tok0 + 512], sg[:], reads=[sg.b], writes=[b_QK])
                    else:
                        sp.dma(PT[di][:, tok0:tok0 + 512], sg[:], reads=[sg.b], writes=[b_PT])
        fw.barrier()

        GR = [(1, 32), (4, 8), (16, 2)]
        with ExitStack() as st:
            qT = [sbuf(st, "qT%d" % i, [128, OWN], BF16) for i in range(2)]
            kT = [sbuf(st, "kT%d" % i, [128, S_LOC], BF16) for i in range(2)]
            vh = [sbuf(st, "vh%d" % i, [128, 32, 128], BF16) for i in range(2)]
            pT = [sbuf(st, "pT%d" % i, [128, 256], BF16) for i in range(3)]
            acc = sbuf(st, "acc", [128, 2, OWN], F32)
            atb = sbuf(st, "atb", [128, OWN], BF16)
            cnt2 = {'it': 0, 'blk': 0, 'gi': 0}
            zt = sbuf(st, "zt", [128, 1024], F32)
            dve.op(lambda e: e.memset(zt[:], 0.0), writes=[zt.b])
            ztb = zt[:, :].bitcast(BF16)
            zero_jobs = []
            for e_ in range(65):
                for hf in range(2):
                    zero_jobs.append((XS[e_ * 128:(e_ + 1) * 128, hf * 2048:(hf + 1) * 2048], ztb, b_XZ))
            for hf in range(2):
                for h2 in range(2):
                    zero_jobs.append((YS[hf][64 * 128:65 * 128, h2 * 1024:(h2 + 1) * 1024], zt[:], b_YS))

            def attn_head(h):
                stages = []
                g0_last = [0]
                for g in range(3):
                    d, nb = GR[g]
                    hd = g * 8 + h
                    q_t, k_t, v_t = qT[cnt2['it'] % 2], kT[cnt2['it'] % 2], vh[cnt2['it'] % 2]
                    cnt2['it'] += 1
                    hb = nb // 2
                    lo = d * 128 * (hb - 1)
                    def loads(q_t=q_t, k_t=k_t, v_t=v_t, hd=hd, lo=lo, d=d, hb=hb, nb=nb):
                        sp.dma(q_t[:], QK[hd][:, OWN:S_LOC], writes=[q_t.b])
                        sp.dma(k_t[:, lo:S_LOC], QK[24 + hd][:, lo:S_LOC], writes=[k_t.b])
                        vsrc = VT[:, hd * 128:(hd + 1) * 128].rearrange("(n i dd) c -> dd i n c", i=128, dd=d)
                        for r in range(d):
                            sp.dma(v_t[:, r * (hb + 1):(r + 1) * (hb + 1), :], vsrc[r][:, hb - 1:nb, :],
                                   writes=[v_t.b])
                    if g < 2:
                        loads()
                    else:
                        stages[g0_last[0]] = stages[g0_last[0]][:2] + (loads,)
                    for r in range(d):
                        for n in range(hb, nb):
                            sb_, ob_ = banks[cnt2['blk'] % 2], banks[2 + cnt2['blk'] % 2]
                            p_t = pT[cnt2['blk'] % 3]
                            cnt2['blk'] += 1
                            mk = maskP if n == hb else maskR
                            qs = r + d * 128 * n - OWN
                            q_ap = q_t[:, qs:qs + d * 127 + 1:d]
                            ks0 = r + d * 128 * (n - 1)
                            ks1 = r + d * 128 * n
                            kp_ap = k_t[:, ks0:ks0 + d * 127 + 1:d]
                            kc_ap = k_t[:, ks1:ks1 + d * 127 + 1:d]
                            vi = r * (hb + 1) + (n - hb)

                            def stage_a(sb_=sb_, mk=mk, kp_ap=kp_ap, kc_ap=kc_ap, q_ap=q_ap, p_t=p_t, k_t=k_t, q_t=q_t):
                                pe.op(lambda e: e.matmul(sb_[:, 0:256], ident_b[:], mk[:], start=True, stop=False),
                                      reads=[ident_b.b, mk.b], writes=[sb_.b])
                                pe.op(lambda e: e.matmul(sb_[:, 0:128], kp_ap, q_ap, start=False, stop=False),
                                      reads=[k_t.b, q_t.b], writes=[sb_.b])
                                pe.op(lambda e: e.matmul(sb_[:, 128:256], kc_ap, q_ap, start=False, stop=True),
                                      reads=[k_t.b, q_t.b], writes=[sb_.b])
                                act.op(lambda e: e.activation(out=p_t[:], in_=sb_[:, 0:256], func=AF.Exp),
                                       reads=[sb_.b], writes=[p_t.b])

                            def stage_b(ob_=ob_, v_t=v_t, vi=vi, p_t=p_t, qs=qs, d=d, g=g):
                                pe.op(lambda e: e.matmul(ob_[:, 0:128], v_t[:, vi, :], p_t[:, 0:128], start=True, stop=False),
                                      reads=[v_t.b, p_t.b], writes=[ob_.b])
                                pe.op(lambda e: e.matmul(ob_[:, 0:128], v_t[:, vi + 1, :], p_t[:, 128:256], start=False, stop=True),
                                      reads=[v_t.b, p_t.b], writes=[ob_.b])
                                pe.op(lambda e: e.matmul(ob_[:, 128:256], ones_b[:], p_t[:, 0:128], start=True, stop=False),
                                      reads=[ones_b.b, p_t.b], writes=[ob_.b])
                                pe.op(lambda e: e.matmul(ob_[:, 128:256], ones_b[:], p_t[:, 128:256], start=False, stop=True),
                                      reads=[ones_b.b, p_t.b], writes=[ob_.b])
                                a_ap = acc[:, :, qs:qs + d * 127 + 1:d]
                                o_ap = ob_[:, 0:256].rearrange("p (a b) -> p a b", a=2)
                                if g == 0:
                                    dve.op(lambda e: e.tensor_copy(out=a_ap, in_=o_ap), reads=[ob_.b], writes=[acc.b])
                                else:
                                    dve.op(lambda e: e.tensor_tensor(out=a_ap, in0=a_ap, in1=o_ap, op=ALU.add),
                                           reads=[ob_.b, acc.b], writes=[acc.b])
                            stages.append((stage_a, stage_b, None))
                    if g == 0:
                        g0_last[0] = len(stages) - 1
                stages[0][0]()
                for i in range(len(stages)):
                    if i + 1 < len(stages):
                        stages[i + 1][0]()
                    stages[i][1]()
                    if stages[i][2] is not None:
                        stages[i][2]()
                    yield
                dve.op(lambda e: e.reciprocal(out=acc[:, 1, :], in_=acc[:, 1, :]), reads=[acc.b], writes=[acc.b])
                dve.op(lambda e: e.tensor_tensor(out=atb[:], in0=acc[:, 0, :], in1=acc[:, 1, :], op=ALU.mult),
                       reads=[acc.b], writes=[atb.b])
                pool.dma(AT[h], atb[:], reads=[atb.b], writes=[b_AT])


            cw = sbuf(st, "cw", [128, 64], F32)
            cb = sbuf(st, "cb", [128, 16], F32)
            bra = sbuf(st, "bra", [128, 16], F32)
            brx = sbuf(st, "brx", [128, 16], F32)
            lm = sbuf(st, "lm", [128, 16], F32)
            spl = sbuf(st, "spl", [128, 16], F32)
            spl2 = sbuf(st, "spl2", [128, 16], F32)
            wga = sbuf(st, "wga", [128, 2, 256], BF16)
            wgx = sbuf(st, "wgx", [128, 2, 256], BF16)
            rx = sbuf(st, "rx", [128, 2, S_LOC], BF16)
            xr = [sbuf(st, "xr%d" % i, [128, S_LOC], F32) for i in range(2)]
            xrb = sbuf(st, "xrb", [128, 2, S_LOC], BF16)
            rr = sbuf(st, "rr", [128, S_LOC], F32)
            ii = sbuf(st, "ii", [128, S_LOC], F32)
            tt = sbuf(st, "tt", [128, S_LOC], F32)
            rgb = sbuf(st, "rgb", [128, OWN], BF16)
            z1 = sbuf(st, "z1", [128, OWN], F32)
            z2 = sbuf(st, "z2", [128, OWN], F32)
            recb = sbuf(st, "recb", [128, OWN], BF16)
            sp.dma(cw[:], convw, writes=[cw.b])
            sp.dma(cb[:], convb, writes=[cb.b])
            sp.dma(bra[:], brga, writes=[bra.b])
            sp.dma(brx[:], brgx, writes=[brx.b])
            sp.dma(lm[:], lam, writes=[lm.b])
            act.op(lambda e: e.activation(out=spl[:], in_=lm[:], func=AF.Exp, scale=-1.0), reads=[lm.b], writes=[spl.b])
            act.op(lambda e: e.activation(out=spl[:], in_=spl[:], func=AF.Ln, bias=1.0), reads=[spl.b], writes=[spl.b])
            dve.op(lambda e: e.tensor_scalar(out=spl2[:], in0=spl[:], scalar1=-16.0, scalar2=None, op0=ALU.mult),
                   reads=[spl.b], writes=[spl2.b])
            dve.op(lambda e: e.tensor_scalar(out=spl[:], in0=spl[:], scalar1=-8.0, scalar2=None, op0=ALU.mult),
                   reads=[spl.b], writes=[spl.b])

            agen = [iter(())]

            def astep(n=1):
                for _ in range(n):
                    try:
                        next(agen[0])
                    except StopIteration:
                        return

            def lru_block(n):
                pool.dma(wga[:], wrga[n].rearrange("p (i j) -> p i j", i=2), writes=[wga.b])
                pool.dma(wgx[:], wrgx[n].rearrange("p (i j) -> p i j", i=2), writes=[wgx.b])
                sp.dma(rx[:], PT[2 * n:2 * n + 2].rearrange("c p t -> p c t"), writes=[rx.b])
                for ic in range(2):
                    c = 2 * n + ic
                    x_ = xr[ic]
                    cv = dve
                    cv.op(lambda e, x_=x_, ic=ic, c=c: e.tensor_scalar(out=x_[:], in0=rx[:, ic, :], scalar1=cw[:, c * 4 + 3:c * 4 + 4],
                                                                       scalar2=cb[:, c:c + 1], op0=ALU.mult, op1=ALU.add),
                           reads=[rx.b, cw.b, cb.b], writes=[x_.b])
                    for k in range(3):
                        s = 3 - k
                        cv.op(lambda e, x_=x_, ic=ic, c=c, k=k, s=s: e.scalar_tensor_tensor(
                            out=x_[:, s:S_LOC], in0=rx[:, ic, 0:S_LOC - s], scalar=cw[:, c * 4 + k:c * 4 + k + 1],
                            in1=x_[:, s:S_LOC], op0=ALU.mult, op1=ALU.add), reads=[rx.b, cw.b, x_.b], writes=[x_.b])
                        astep(3)
                    act.op(lambda e, x_=x_, ic=ic: e.activation(out=xrb[:, ic, :], in_=x_[:], func=AF.Copy),
                           reads=[x_.b], writes=[xrb.b])
                for jc in range(2):
                    c = 2 * n + jc
                    x_ = xr[jc]
                    sp.dma(rgb[:], PT[16 + c][:, OWN:S_LOC], writes=[rgb.b])
                    act.op(lambda e: e.activation(out=z1[:], in_=rgb[:], func=AF.Copy), reads=[rgb.b], writes=[z1.b])
                    pool.op(lambda e: e.tensor_tensor(out=z2[:], in0=z1[:], in1=z1[:], op=ALU.mult), reads=[z1.b], writes=[z2.b])
                    pool.op(lambda e: e.tensor_scalar(out=z2[:], in0=z2[:], scalar1=0.044715, scalar2=1.0, op0=ALU.mult, op1=ALU.add),
                            reads=[z2.b], writes=[z2.b])
                    dve.op(lambda e: e.tensor_tensor(out=z2[:], in0=z2[:], in1=z1[:], op=ALU.mult), reads=[z2.b, z1.b], writes=[z2.b])
                    act.op(lambda e: e.activation(out=z2[:], in_=z2[:], func=AF.Sigmoid, scale=1.5957691216057308),
                           reads=[z2.b], writes=[z2.b])
                    dve.op(lambda e: e.tensor_tensor(out=z2[:], in0=z2[:], in1=z1[:], op=ALU.mult), reads=[z2.b, z1.b], writes=[z2.b])
                    for (wgt, bias_t, dst) in ((wga, bra, rr), (wgx, brx, ii)):
                        for tl in range(8):
                            bk = banks[4 + cnt2['gi'] % 4]
                            cnt2['gi'] += 1
                            for ic in range(2):
                                pe.op(lambda e, bk=bk, wgt=wgt, ic=ic, jc=jc, tl=tl: e.matmul(
                                    bk[:], wgt[:, ic, jc * 128:(jc + 1) * 128], xrb[:, ic, tl * 512:(tl + 1) * 512],
                                    start=(ic == 0), stop=(ic == 1)), reads=[wgt.b, xrb.b], writes=[bk.b])
                            act.op(lambda e, bk=bk, dst=dst, bias_t=bias_t, c=c, tl=tl: e.activation(
                                out=dst[:, tl * 512:(tl + 1) * 512], in_=bk[:], func=AF.Sigmoid, bias=bias_t[:, c:c + 1]),
                                reads=[bk.b, bias_t.b], writes=[dst.b])
                    astep(8)
                    act.op(lambda e, c=c: e.activation(out=tt[:], in_=rr[:], func=AF.Exp, scale=spl2[:, c:c + 1]),
                           reads=[rr.b, spl2.b], writes=[tt.b])
                    act.op(lambda e, c=c: e.activation(out=rr[:], in_=rr[:], func=AF.Exp, scale=spl[:, c:c + 1]),
                           reads=[rr.b, spl.b], writes=[rr.b])
                    act.op(lambda e: e.activation(out=tt[:], in_=tt[:], func=AF.Sqrt, scale=-1.0, bias=1.0),
                           reads=[tt.b], writes=[tt.b])
                    dve.op(lambda e, x_=x_: e.tensor_tensor(out=ii[:], in0=ii[:], in1=x_[:], op=ALU.mult),
                           reads=[ii.b, x_.b], writes=[ii.b])
                    dve.op(lambda e: e.scalar_tensor_tensor(out=ii[:, 0:OWN], in0=ii[:, 0:OWN], scalar=flag[:, 0:1],
                                                            in1=tt[:, 0:OWN], op0=ALU.mult, op1=ALU.mult),
                           reads=[ii.b, tt.b, flag.b], writes=[ii.b])
                    dve.op(lambda e: e.tensor_tensor(out=ii[:, OWN:S_LOC], in0=ii[:, OWN:S_LOC], in1=tt[:, OWN:S_LOC], op=ALU.mult),
                           reads=[ii.b, tt.b], writes=[ii.b])
                    dve.op(lambda e: e.tensor_tensor_scan(out=tt[:], data0=rr[:], data1=ii[:], initial=0.0,
                                                          op0=ALU.mult, op1=ALU.add), reads=[rr.b, ii.b], writes=[tt.b])
                    dve.op(lambda e: e.tensor_tensor(out=recb[:], in0=z2[:], in1=tt[:, OWN:S_LOC], op=ALU.mult),
                           reads=[z2.b, tt.b], writes=[recb.b])
                    pool.dma(RT[c], recb[:], reads=[recb.b], writes=[b_RT])
            for h in range(8):
                agen[0] = attn_head(h)
                astep(2)
                lru_block(h)
                for _ in agen[0]:
                    pass
                for i in range(4 * h, 4 * h + 4):
                    pool.dma(WPB[i], wpr[i], writes=[b_AT])
                    pool.dma(WOB[i], wout[i], writes=[b_AT])
                for (zo, zi, zb) in zero_jobs[h * 17:(h + 1) * 17]:
                    sp.dma(zo, zi, reads=[zt.b], writes=[zb])
        fw.barrier()

        def layer_norm_gen(res, gt, bt, junk, st1, st2, badd=None):
            badd = badd or pool
            dve.op(lambda e: e.reduce_sum(out=st1[:, 0:1], in_=res[:], axis=AX.X), reads=[res.b], writes=[st1.b])
            dve.op(lambda e: e.tensor_scalar(out=st1[:, 1:2], in0=st1[:, 0:1], scalar1=-1.0 / D, scalar2=None, op0=ALU.mult),
                   reads=[st1.b], writes=[st1.b])
            yield
            act.op(lambda e: e.activation(out=junk[:], in_=res[:], func=AF.Square, bias=st1[:, 1:2], accum_out=st2[:, 0:1]),
                   reads=[res.b, st1.b], writes=[junk.b, st2.b])
            act.op(lambda e: e.activation(out=st2[:, 1:2], in_=st2[:, 0:1], func=AF.Sqrt, scale=1.0 / D, bias=EPS),
                   reads=[st2.b], writes=[st2.b])
            yield
            dve.op(lambda e: e.reciprocal(out=st2[:, 2:3], in_=st2[:, 1:2]), reads=[st2.b], writes=[st2.b])
            dve.op(lambda e: e.tensor_tensor(out=st2[:, 3:4], in0=st1[:, 1:2], in1=st2[:, 2:3], op=ALU.mult),
                   reads=[st1.b, st2.b], writes=[st2.b])
            act.op(lambda e: e.activation(out=res[:], in_=res[:], func=AF.Identity, scale=st2[:, 2:3], bias=st2[:, 3:4]),
                   reads=[res.b, st2.b], writes=[res.b])
            yield
            dve.op(lambda e: e.tensor_tensor(out=res[:], in0=res[:], in1=gt[:], op=ALU.mult), reads=[res.b, gt.b], writes=[res.b])
            badd.op(lambda e: e.tensor_tensor(out=res[:], in0=res[:], in1=bt[:], op=ALU.add), reads=[res.b, bt.b], writes=[res.b])
            yield

        def layer_norm(res, gt, bt, junk, st1, st2, badd=None):
            for _ in layer_norm_gen(res, gt, bt, junk, st1, st2, badd):
                pass

        with ExitStack() as st:
            ring = Ring(st, 5, "r3_")
            G1 = sbuf(st, "G1", [128, D], F32)
            B1 = sbuf(st, "B1", [128, D], F32)
            wr_s = sbuf(st, "wr_s", [128, 32, 72], F32)
            br_s = sbuf(st, "br_s", [128, 72], F32)
            att = sbuf(st, "att", [128, 8, 256], BF16)
            rect = sbuf(st, "rect", [128, 16, 256], BF16)
            mrg = sbuf(st, "mrg", [128, 32, 256], BF16)
            gts = [sbuf(st, "gts%d" % i, [128, 2, 256], BF16) for i in range(2)]
            t1 = sbuf(st, "t1", [128, 256], F32)
            t2 = sbuf(st, "t2", [128, 256], F32)
            res = [[sbuf(st, "res%d_%d" % (a, i), [128, D], F32) for i in range(2)] for a in range(2)]
            h1bs = [sbuf(st, "h1b%d" % i, [128, D], BF16) for i in range(2)]
            h1Tg = [sbuf(st, "h1Tg%d" % i, [128, 4, 128], F32) for i in range(2)]
            st1 = sbuf(st, "st1", [128, 2], F32)
            st2 = sbuf(st, "st2", [128, 4], F32)
            lgt = sbuf(st, "lgt", [128, 72], F32)
            sm = sbuf(st, "sm", [128, 16], F32)
            ohg = sbuf(st, "ohg", [128, 8], F32)
            tmp64 = sbuf(st, "tmp64", [128, 64], F32)
            les = sbuf(st, "les", [128, 8], F32)
            le2 = sbuf(st, "le2", [128, 8], F32)
            oh1 = sbuf(st, "oh1", [128, 8], F32)
            oh2 = sbuf(st, "oh2", [128, 8], F32)
            OH = [sbuf(st, "OH%d" % i, [128, 64], F32) for i in range(2)]
            Mb = sbuf(st, "Mb", [128, 64], BF16)
            runc = sbuf(st, "runc", [128, 64], F32)
            pb = sbuf(st, "pb", [128, 64], F32)
            dst_f = sbuf(st, "dst_f", [128, 4], F32)

            sp.dma(G1[:], ln1g[0:1, :].partition_broadcast(128), writes=[G1.b])
            sp.dma(B1[:], ln1b[0:1, :].partition_broadcast(128), writes=[B1.b])
            sp.dma(wr_s[:], wrt.rearrange("p (k c) -> p k c", k=32), writes=[wr_s.b])
            sp.dma(br_s[:], brt[0:1, :].partition_broadcast(128), writes=[br_s.b])
            dve.op(lambda e: e.memset(runc[:], 0.0), writes=[runc.b])

            def epi_a(rs, sub):
                h1b = h1bs[sub % 2]
                for _ in layer_norm_gen(rs, G1, B1, h1b, st1, st2, badd=dve):
                    yield
                sp.dma(H1[sub * 128:(sub + 1) * 128, :], rs[:], reads=[rs.b], writes=[b_H1])
                act.op(lambda e: e.activation(out=h1b[:], in_=rs[:], func=AF.Copy), reads=[rs.b], writes=[h1b.b])
                yield

            def epi_bc(rs, sub):
                h1b = h1bs[sub % 2]
                lb = banks[3]
                for k4 in range(8):
                    hg = h1Tg[k4 % 2]
                    tb = banks[k4 % 3]
                    for i in range(4):
                        k = k4 * 4 + i
                        pe.op(lambda e, k=k, i=i, tb=tb: e.transpose(tb[:, i * 128:(i + 1) * 128], rs[:, k * 128:(k + 1) * 128], ident_f[:]),
                              reads=[rs.b, ident_f.b], writes=[tb.b])
                    act.op(lambda e, hg=hg, tb=tb: e.activation(out=hg[:], in_=tb[:, :].rearrange("p (a b) -> p a b", a=4), func=AF.Copy),
                           reads=[tb.b], writes=[hg.b])
                    for i in range(4):
                        k = k4 * 4 + i
                        pe.op(lambda e, k=k, i=i, hg=hg: e.matmul(lb[:, 0:72], hg[:, i, :], wr_s[:, k, :], start=(k == 0), stop=(k == 31)),
                              reads=[hg.b, wr_s.b], writes=[lb.b])
                    yield
                dve.op(lambda e: e.tensor_tensor(out=lgt[:], in0=lb[:, 0:72], in1=br_s[:], op=ALU.add),
                       reads=[lb.b, br_s.b], writes=[lgt.b])
                dve.op(lambda e: e.reduce_max(out=sm[:, 0:1], in_=lgt[:, 0:8], axis=AX.X), reads=[lgt.b], writes=[sm.b])
                dve.op(lambda e: e.tensor_scalar(out=ohg[:], in0=lgt[:, 0:8], scalar1=sm[:, 0:1], scalar2=None, op0=ALU.is_equal),
                       reads=[lgt.b, sm.b], writes=[ohg.b])
                dve.op(lambda e: e.tensor_scalar(out=sm[:, 1:2], in0=sm[:, 0:1], scalar1=-1.0, scalar2=None, op0=ALU.mult),
                       reads=[sm.b], writes=[sm.b])
                yield
                act.op(lambda e: e.activation(out=les[:], in_=lgt[:, 0:8], func=AF.Exp, bias=sm[:, 1:2], accum_out=sm[:, 2:3]),
                       reads=[lgt.b, sm.b], writes=[les.b, sm.b])
                dve.op(lambda e: e.reciprocal(out=sm[:, 3:4], in_=sm[:, 2:3]), reads=[sm.b], writes=[sm.b])
                dve.op(lambda e: e.tensor_tensor(out=tmp64[:, :].rearrange("p (g j) -> p g j", g=8),
                                                 in0=lgt[:, 8:72].rearrange("p (g j) -> p g j", g=8),
                                                 in1=ohg[:, :].unsqueeze(2).broadcast_to([128, 8, 8]), op=ALU.mult),
                       reads=[lgt.b, ohg.b], writes=[tmp64.b])
                dve.op(lambda e: e.reduce_sum(out=les[:], in_=tmp64[:, :].rearrange("p (g j) -> p j g", g=8), axis=AX.X),
                       reads=[tmp64.b], writes=[les.b])
                yield
                dve.op(lambda e: e.reduce_max(out=sm[:, 4:5], in_=les[:], axis=AX.X), reads=[les.b], writes=[sm.b])
                dve.op(lambda e: e.tensor_scalar(out=oh1[:], in0=les[:], scalar1=sm[:, 4:5], scalar2=None, op0=ALU.is_equal),
                       reads=[les.b, sm.b], writes=[oh1.b])
                dve.op(lambda e: e.scalar_tensor_tensor(out=le2[:], in0=oh1[:], scalar=-1e30, in1=les[:], op0=ALU.mult, op1=ALU.add),
                       reads=[oh1.b, les.b], writes=[le2.b])
                dve.op(lambda e: e.reduce_max(out=sm[:, 5:6], in_=le2[:], axis=AX.X), reads=[le2.b], writes=[sm.b])
                yield
                dve.op(lambda e: e.tensor_scalar(out=oh2[:], in0=le2[:], scalar1=sm[:, 5:6], scalar2=None, op0=ALU.is_equal),
                       reads=[le2.b, sm.b], writes=[oh2.b])
                dve.op(lambda e: e.tensor_scalar(out=sm[:, 6:7], in0=sm[:, 4:5], scalar1=-1.0, scalar2=None, op0=ALU.mult),
                       reads=[sm.b], writes=[sm.b])
                act.op(lambda e: e.activation(out=sm[:, 7:8], in_=sm[:, 5:6], func=AF.Exp, bias=sm[:, 6:7]),
                       reads=[sm.b], writes=[sm.b])
                dve.op(lambda e: e.tensor_scalar(out=sm[:, 8:9], in0=sm[:, 7:8], scalar1=1.0, scalar2=None, op0=ALU.add),
                       reads=[sm.b], writes=[sm.b])
                yield
                dve.op(lambda e: e.reciprocal(out=sm[:, 9:10], in_=sm[:, 8:9]), reads=[sm.b], writes=[sm.b])
                dve.op(lambda e: e.tensor_tensor(out=sm[:, 10:11], in0=sm[:, 9:10], in1=sm[:, 7:8], op=ALU.mult),
                       reads=[sm.b], writes=[sm.b])
                dve.op(lambda e: e.tensor_tensor(out=wts[:, 2 * sub:2 * sub + 2], in0=sm[:, 9:11],
                                                 in1=sm[:, 3:4].broadcast_to([128, 2]), op=ALU.mult),
                       reads=[sm.b], writes=[wts.b])
                for kk, oh in enumerate((oh1, oh2)):
                    dve.op(lambda e, kk=kk, oh=oh: e.tensor_tensor(
                        out=OH[kk][:, :].rearrange("p (g j) -> p g j", g=8),
                        in0=ohg[:, :].unsqueeze(2).broadcast_to([128, 8, 8]),
                        in1=oh[:, :].unsqueeze(1).broadcast_to([128, 8, 8]), op=ALU.mult),
                        reads=[ohg.b, oh.b], writes=[OH[kk].b])
                yield
                dve.op(lambda e: e.tensor_tensor(out=Mb[:], in0=OH[0][:], in1=OH[1][:], op=ALU.add),
                       reads=[OH[0].b, OH[1].b], writes=[Mb.b])
                pe.op(lambda e: e.matmul(lb[:, 128:192], ustr[:], Mb[:], start=True, stop=True),
                      reads=[ustr.b, Mb.b], writes=[lb.b])
                pe.op(lambda e: e.matmul(lb[:, 192:256], ones_b[:], Mb[:], start=True, stop=True),
                      reads=[ones_b.b, Mb.b], writes=[lb.b])
                dve.op(lambda e: e.tensor_tensor(out=pb[:], in0=lb[:, 128:192], in1=runc[:], op=ALU.add),
                       reads=[lb.b, runc.b], writes=[pb.b])
                dve.op(lambda e: e.tensor_tensor(out=runc[:], in0=lb[:, 192:256], in1=runc[:], op=ALU.add),
                       reads=[lb.b, runc.b], writes=[runc.b])
                yield
                for kk in range(2):
                    dve.op(lambda e, kk=kk: e.tensor_tensor(out=tmp64[:], in0=OH[kk][:], in1=pb[:], op=ALU.mult),
                           reads=[OH[kk].b, pb.b], writes=[tmp64.b])
                    dve.op(lambda e: e.reduce_sum(out=dst_f[:, 0:1], in_=tmp64[:], axis=AX.X), reads=[tmp64.b], writes=[dst_f.b])
                    dve.op(lambda e, kk=kk: e.tensor_tensor(out=tmp64[:], in0=OH[kk][:], in1=e128[:], op=ALU.mult),
                           reads=[OH[kk].b, e128.b], writes=[tmp64.b])
                    dve.op(lambda e: e.reduce_sum(out=dst_f[:, 1:2], in_=tmp64[:], axis=AX.X), reads=[tmp64.b], writes=[dst_f.b])
                    yield
                    dve.op(lambda e: e.tensor_scalar(out=dst_f[:, 2:3], in0=dst_f[:, 0:1], scalar1=128.0, scalar2=None, op0=ALU.is_ge),
                           reads=[dst_f.b], writes=[dst_f.b])
                    dve.op(lambda e: e.tensor_tensor(out=dst_f[:, 1:2], in0=dst_f[:, 1:2], in1=dst_f[:, 0:1], op=ALU.add),
                           reads=[dst_f.b], writes=[dst_f.b])
                    dve.op(lambda e: e.tensor_scalar(out=dst_f[:, 3:4], in0=dst_f[:, 2:3], scalar1=-1.0, scalar2=1.0, op0=ALU.mult, op1=ALU.add),
                           reads=[dst_f.b], writes=[dst_f.b])
                    dve.op(lambda e: e.tensor_tensor(out=dst_f[:, 1:2], in0=dst_f[:, 1:2], in1=dst_f[:, 3:4], op=ALU.mult),
                           reads=[dst_f.b], writes=[dst_f.b])
                    yield
                    dve.op(lambda e: e.scalar_tensor_tensor(out=dst_f[:, 1:2], in0=dst_f[:, 2:3], scalar=dumpc[:, 0:1], in1=dst_f[:, 1:2],
                                                            op0=ALU.mult, op1=ALU.add), reads=[dst_f.b, dumpc.b], writes=[dst_f.b])
                    col = 2 * sub + kk
                    dve.op(lambda e, col=col: e.tensor_tensor(out=wts[:, col:col + 1], in0=wts[:, col:col + 1], in1=dst_f[:, 3:4], op=ALU.mult),
                           reads=[dst_f.b, wts.b], writes=[wts.b])
                    dve.op(lambda e, col=col: e.tensor_copy(out=destI[:, col:col + 1], in_=dst_f[:, 1:2]),
                           reads=[dst_f.b], writes=[destI.b])
                    pool.op(lambda e, col=col: e.indirect_dma_start(
                        out=XS[:, :], out_offset=bass.IndirectOffsetOnAxis(ap=destI[:, col:col + 1], axis=0),
                        in_=h1b[:, :], in_offset=None),
                        reads=[h1b.b, destI.b, b_XZ], writes=[b_XS], is_dma=True)
                    yield

            epi = [iter(())]
            epi_ln = [iter(())]

            def step(n=1, g=None):
                g = g or epi
                for _ in range(n):
                    try:
                        next(g[0])
                    except StopIteration:
                        return

            def drain():
                for _ in epi_ln[0]:
                    pass
                for _ in epi[0]:
                    pass

            def chain2(a, b):
                for _ in a:
                    yield
                for _ in b:
                    yield

            gcnt = 0
            for tile in range(8):
                t0 = tile * 256
                rs2 = res[tile % 2]
                sp.dma(att[:], AT[:, :, t0:t0 + 256].rearrange("c p t -> p c t"), writes=[att.b])
                sp.dma(rect[:], RT[:, :, t0:t0 + 256].rearrange("c p t -> p c t"), writes=[rect.b])
                for s_ in range(2):
                    sp.dma(rs2[s_][:], xown[t0 + s_ * 128:t0 + (s_ + 1) * 128, :], writes=[rs2[s_].b])
                for j in range(32):
                    pa = ring.load(WPB[j], 3072)
                    pav = pa[:, 0:1024].rearrange("p (k c) -> p k c", k=8)
                    prv = pa[:, 1024:3072].rearrange("p (k c) -> p k c", k=16)
                    yb_ = banks[gcnt % 2]
                    gt = gts[gcnt % 2]
                    gcnt += 1
                    pool.dma(gt[:], PT[32 + j:96:32][:, :, OWN + t0:OWN + t0 + 256].rearrange("g p t -> p g t"),
                             writes=[gt.b])
                    for k in range(8):
                        pe.op(lambda e, yb_=yb_, pav=pav, k=k: e.matmul(yb_[:, 0:256], pav[:, k, :], att[:, k, :], start=(k == 0), stop=(k == 7)),
                              reads=[pa.b, att.b], writes=[yb_.b])
                    for k in range(16):
                        pe.op(lambda e, yb_=yb_, prv=prv, k=k: e.matmul(yb_[:, 256:512], prv[:, k, :], rect[:, k, :], start=(k == 0), stop=(k == 15)),
                              reads=[pa.b, rect.b], writes=[yb_.b])
                    dve.op(lambda e, gt=gt, yb_=yb_: e.tensor_tensor(out=t1[:], in0=gt[:, 0, :], in1=yb_[:, 0:256], op=ALU.mult),
                           reads=[gt.b, yb_.b], writes=[t1.b])
                    dve.op(lambda e, gt=gt, yb_=yb_: e.tensor_tensor(out=t2[:], in0=gt[:, 1, :], in1=yb_[:, 256:512], op=ALU.mult),
                           reads=[gt.b, yb_.b], writes=[t2.b])
                    dve.op(lambda e, j=j: e.tensor_tensor(out=mrg[:, j, :], in0=t1[:], in1=t2[:], op=ALU.add),
                           reads=[t1.b, t2.b], writes=[mrg.b])
                    if j % 3 == 2:
                        step(1, epi_ln)
                for _ in epi_ln[0]:
                    pass
                for eg in range(8):
                    for q in range(4):
                        po = ring.load(WOB[eg * 4 + q])
                        pov = po[:, :].rearrange("p (k e) -> p k e", k=8)
                        for s_ in range(2):
                            bk = banks[4 + 2 * (eg % 2) + s_]
                            for kk in range(8):
                                pe.op(lambda e, bk=bk, pov=pov, q=q, kk=kk, s_=s_: e.matmul(
                                    bk[:], mrg[:, q * 8 + kk, s_ * 128:(s_ + 1) * 128], pov[:, kk, :],
                                    start=(q == 0 and kk == 0), stop=(q == 3 and kk == 7)),
                                    reads=[po.b, mrg.b], writes=[bk.b])
                        step(2 if (eg * 4 + q) % 4 == 0 else 1)
                    for s_ in range(2):
                        bk = banks[4 + 2 * (eg % 2) + s_]
                        rs = rs2[s_]
                        dve.op(lambda e, bk=bk, rs=rs, eg=eg: e.scalar_tensor_tensor(
                            out=rs[:, eg * 512:(eg + 1) * 512], in0=rs[:, eg * 512:(eg + 1) * 512], scalar=ALPHA,
                            in1=bk[:], op0=ALU.mult, op1=ALU.add), reads=[bk.b, rs.b], writes=[rs.b])
                drain()
                epi_ln[0] = chain2(epi_a(rs2[0], tile * 2), epi_a(rs2[1], tile * 2 + 1))
                epi[0] = chain2(epi_bc(rs2[0], tile * 2), epi_bc(rs2[1], tile * 2 + 1))
            drain()
        fw.barrier()

        with ExitStack() as st:
            ring = Ring(st, 14, "r4_")
            xe = [sbuf(st, "xe%d" % i, [128, D], BF16) for i in range(2)]
            xeT = [sbuf(st, "xeT%d" % i, [128, 32, 128], BF16) for i in range(2)]
            sg_t = sbuf(st, "sg_t", [128, 512], F32)
            hid = [sbuf(st, "hid%d" % i, [128, 512], BF16) for i in range(2)]
            ye = [sbuf(st, "ye%d" % i, [128, D], F32) for i in range(2)]
            ev = 0
            for e_ in range(64):
                x_e, x_T, hd_, y_e = xe[e_ % 2], xeT[e_ % 2], hid[e_ % 2], ye[e_ % 2]
                if e_ == 0:
                    sp.dma(x_e[:], XS[0:128, :], writes=[x_e.b])
                for k8 in range(4):
                    bk = banks[k8 % 2]
                    bkb = bk[:, :].bitcast(BF16)
                    for i in range(8):
                        k = k8 * 8 + i
                        pe.op(lambda e, bkb=bkb, x_e=x_e, k=k, i=i: e.transpose(bkb[:, i * 128:(i + 1) * 128], x_e[:, k * 128:(k + 1) * 128], ident_b[:]),
                              reads=[x_e.b, ident_b.b], writes=[bk.b])
                    act.op(lambda e, bkb=bkb, x_T=x_T, k8=k8: e.activation(
                        out=x_T[:, k8 * 8:(k8 + 1) * 8, :], in_=bkb.rearrange("p (a b) -> p a b", a=8), func=AF.Copy),
                        reads=[bk.b], writes=[x_T.b])
                gb, ub = banks[2], banks[3]
                for q in range(4):
                    pg = ring.load(wg[e_][q])
                    pu = ring.load(wu[e_][q])
                    for (pp, bk) in ((pg, gb), (pu, ub)):
                        ppv = pp[:, :].rearrange("p (k f) -> p k f", k=8)
                        for fc in range(4):
                            for kk in range(8):
                                pe.op(lambda e, bk=bk, ppv=ppv, fc=fc, kk=kk, q=q, x_T=x_T: e.matmul(
                                    bk[:, fc * 128:(fc + 1) * 128], ppv[:, kk, fc * 128:(fc + 1) * 128], x_T[:, q * 8 + kk, :],
                                    start=(q == 0 and kk == 0 and fc == 0), stop=(q == 3 and kk == 7 and fc == 3),
                                    skip_group_check=True),
                                    reads=[pp.b, x_T.b], writes=[bk.b])
                act.op(lambda e, gb=gb: e.activation(out=sg_t[:], in_=gb[:], func=AF.Silu), reads=[gb.b], writes=[sg_t.b])
                dve.op(lambda e, ub=ub, hd_=hd_: e.tensor_tensor(out=hd_[:], in0=sg_t[:], in1=ub[:], op=ALU.mult),
                       reads=[sg_t.b, ub.b], writes=[hd_.b])
                pds = [ring.load(wd[e_][fc * 128:(fc + 1) * 128, :]) for fc in range(4)]
                for dg in range(8):
                    bk = banks[4 + dg % 4]
                    for fc in range(4):
                        pe.op(lambda e, bk=bk, fc=fc, dg=dg, hd_=hd_, pd=pds[fc]: e.matmul(
                            bk[:], hd_[:, fc * 128:(fc + 1) * 128], pd[:, dg * 512:(dg + 1) * 512], start=(fc == 0), stop=(fc == 3)),
                            reads=[hd_.b, pds[fc].b], writes=[bk.b])
                    if ev % 2 == 0:
                        act.op(lambda e, bk=bk, y_e=y_e, dg=dg: e.activation(out=y_e[:, dg * 512:(dg + 1) * 512], in_=bk[:], func=AF.Copy),
                               reads=[bk.b], writes=[y_e.b])
                    else:
                        dve.op(lambda e, bk=bk, y_e=y_e, dg=dg: e.tensor_copy(out=y_e[:, dg * 512:(dg + 1) * 512], in_=bk[:]),
                               reads=[bk.b], writes=[y_e.b])
                    ev += 1
                if e_ + 1 < 64:
                    xn = xe[(e_ + 1) % 2]
                    sp.dma(xn[:], XS[(e_ + 1) * 128:(e_ + 2) * 128, :], writes=[xn.b])
                for hf in range(2):
                    sp.dma(YS[hf][e_ * 128:(e_ + 1) * 128, :], y_e[:, hf * 2048:(hf + 1) * 2048], reads=[y_e.b], writes=[b_YS])
        fw.barrier()

        with ExitStack() as st:
            G2 = sbuf(st, "G2", [128, D], F32)
            B2 = sbuf(st, "B2", [128, D], F32)
            hr = [sbuf(st, "hr%d" % i, [128, D], F32) for i in range(2)]
            ya = [sbuf(st, "ya%d" % i, [128, D], F32) for i in range(2)]
            yb = [sbuf(st, "yb%d" % i, [128, D], F32) for i in range(2)]
            junk = sbuf(st, "junk", [128, D], BF16)
            st1 = sbuf(st, "st1b", [128, 2], F32)
            st2 = sbuf(st, "st2b", [128, 4], F32)
            sp.dma(G2[:], ln2g[0:1, :].partition_broadcast(128), writes=[G2.b])
            sp.dma(B2[:], ln2b[0:1, :].partition_broadcast(128), writes=[B2.b])
            for i in range(2):
                dve.op(lambda e, i=i: e.memset(ya[i][:], 0.0), writes=[ya[i].b])
                dve.op(lambda e, i=i: e.memset(yb[i][:], 0.0), writes=[yb[i].b])
            def fetch(sub):
                h_, a_, b_ = hr[sub % 2], ya[sub % 2], yb[sub % 2]
                sp.dma(h_[:], H1[sub * 128:(sub + 1) * 128, :], writes=[h_.b])
                for (dstt, col) in ((a_, 2 * sub), (b_, 2 * sub + 1)):
                    for hf in range(2):
                        pool.op(lambda e, dstt=dstt, col=col, hf=hf: e.indirect_dma_start(
                            out=dstt[:, hf * 2048:(hf + 1) * 2048], out_offset=None, in_=YS[hf][:, :],
                            in_offset=bass.IndirectOffsetOnAxis(ap=destI[:, col:col + 1], axis=0)),
                            reads=[destI.b], writes=[dstt.b], is_dma=True)

            fetch(0)
            for sub in range(16):
                h_, a_, b_ = hr[sub % 2], ya[sub % 2], yb[sub % 2]
                act.op(lambda e, h_=h_: e.activation(out=h_[:], in_=h_[:], func=AF.Identity, scale=ALPHA), reads=[h_.b], writes=[h_.b])
                dve.op(lambda e, h_=h_, a_=a_, sub=sub: e.scalar_tensor_tensor(out=h_[:], in0=a_[:], scalar=wts[:, 2 * sub:2 * sub + 1],
                                                                              in1=h_[:], op0=ALU.mult, op1=ALU.add),
                       reads=[a_.b, h_.b, wts.b], writes=[h_.b])
                dve.op(lambda e, h_=h_, b_=b_, sub=sub: e.scalar_tensor_tensor(out=h_[:], in0=b_[:], scalar=wts[:, 2 * sub + 1:2 * sub + 2],
                                                                              in1=h_[:], op0=ALU.mult, op1=ALU.add),
                       reads=[b_.b, h_.b, wts.b], writes=[h_.b])
                if sub + 1 < 16:
                    fetch(sub + 1)
                layer_norm(h_, G2, B2, junk, st1, st2, badd=dve)
                pool.dma(out[sub * 128:(sub + 1) * 128, :], h_[:], reads=[h_.b], writes=[b_out])

        fw.emit(top)
    return nc


def _consts(half):
    ident = np.eye(128, dtype=np.float32)
    NEG = -30000.0
    k = np.arange(128)[:, None]
    q = np.arange(128)[None, :]
    mprev = np.where(k >= q, 0.0, NEG).astype(np.float32)
    mcur = np.where(k <= q, 0.0, NEG).astype(np.float32)
    maskR = np.concatenate([mprev, mcur], axis=1)
    maskP = maskR.copy()
    if half == 0:
        maskP[:, :128] = NEG
    pm = np.zeros((32, 32), np.float32)
    for m in range(16):
        pm[m + 16, m] = -1.0
    for m in range(16, 32):
        pm[m - 16, m] = 1.0
    invf = np.power(np.float32(500000.0), -np.arange(16, dtype=np.float32) * np.float32(2.0) / np.float32(32)).astype(np.float32)
    invf = np.concatenate([invf, invf]).reshape(32, 1)
    flag = np.full((128, 1), float(half), np.float32)
    ustr = (np.arange(128)[:, None] < np.arange(128)[None, :]).astype(np.float32)
    e128 = np.tile((np.arange(64, dtype=np.float32) * 128.0)[None, :], (128, 1))
    dump = (8192.0 + np.arange(128, dtype=np.float32)).reshape(128, 1)
    return dict(c_dump=dump, c_ident=ident, c_maskR=maskR, c_maskP=maskP, c_pm=pm, c_invf=invf, c_flag=flag, c_ustr=ustr, c_e128=e128)


def _prep_shared(inp):
    f = np.float32
    sh = {}
    w_in = np.asarray(inp["w_in"], f)[0]
    sh["win"] = np.ascontiguousarray(w_in.reshape(32, 128, NCH_IN, 128).transpose(2, 1, 0, 3)).reshape(NCH_IN, 128, 4096)
    bgt = np.asarray(inp["b_gate"], f)[0]
    sh["bgate"] = np.ascontiguousarray(bgt.reshape(2, 32, 128).transpose(2, 0, 1)).reshape(128, 64)
    cw = np.asarray(inp["conv_w"], f)[0]
    sh["convw"] = np.ascontiguousarray(cw.reshape(4, 16, 128).transpose(2, 1, 0)).reshape(128, 64)
    sh["convb"] = np.ascontiguousarray(np.asarray(inp["conv_b"], f)[0].reshape(16, 128).T)
    for nm, key in (("wrga", "w_rg_a"), ("wrgx", "w_rg_x")):
        w = np.asarray(inp[key], f)[0]
        sh[nm] = np.ascontiguousarray(w.reshape(8, 2, 128, 256).transpose(0, 2, 1, 3)).reshape(8, 128, 512)
    for nm, key in (("brga", "b_rg_a"), ("brgx", "b_rg_x")):
        sh[nm] = np.ascontiguousarray(np.asarray(inp[key], f)[0].reshape(16, 128).T)
    sh["lam"] = np.ascontiguousarray(np.asarray(inp["lru_lambda"], f)[0].reshape(16, 128).T)
    wa = np.asarray(inp["w_attn_proj"], f)[0]
    wa_l = wa.reshape(8, 128, 32, 128).transpose(2, 1, 0, 3).reshape(32, 128, 1024)
    wr = np.asarray(inp["w_rec_proj"], f)[0]
    wr_l = wr.reshape(16, 128, 32, 128).transpose(2, 1, 0, 3).reshape(32, 128, 2048)
    sh["wpr"] = np.ascontiguousarray(np.concatenate([wa_l, wr_l], axis=2))
    wo = np.asarray(inp["w_out"], f)[0]
    sh["wout"] = np.ascontiguousarray(wo.reshape(4, 8, 128, 8, 512).transpose(3, 0, 2, 1, 4)).reshape(32, 128, 4096)
    sh["ln1g"] = np.asarray(inp["ln1_g"], f).reshape(1, D)
    sh["ln1b"] = np.asarray(inp["ln1_b"], f).reshape(1, D)
    wrt = np.concatenate([np.asarray(inp["w_router_group"], f)[0], np.asarray(inp["w_router_expert"], f)[0]], axis=1)
    sh["wrt"] = np.ascontiguousarray(wrt.reshape(32, 128, 72).transpose(1, 0, 2)).reshape(128, 32 * 72)
    sh["brt"] = np.concatenate([np.asarray(inp["b_router_group"], f)[0], np.asarray(inp["b_router_expert"], f)[0]]).reshape(1, 72)
    for nm, key in (("wg", "w_gate"), ("wu", "w_up")):
        w = np.asarray(inp[key], f)[0]
        sh[nm] = np.ascontiguousarray(w.reshape(64, 4, 8, 128, 512).transpose(0, 1, 3, 2, 4)).reshape(64, 4, 128, 4096)
    sh["wd"] = np.asarray(inp["w_down"], f)[0]
    sh["ln2g"] = np.asarray(inp["ln2_g"], f).reshape(1, D)
    sh["ln2b"] = np.asarray(inp["ln2_b"], f).reshape(1, D)
    return sh


def kernel(**inp):
    x = np.asarray(inp["x"], np.float32)
    positions = np.asarray(inp["positions"], np.int32)
    sh = _prep_shared(inp)
    in_maps = []
    for c in range(8):
        b, half = c // 2, c % 2
        xl = np.zeros((S_LOC, D), np.float32)
        pl = np.zeros((1, S_LOC), np.int32)
        if half == 1:
            xl[:] = x[b]
            pl[0] = positions[b]
        else:
            xl[OWN:] = x[b, :OWN]
            pl[0, OWN:] = positions[b, :OWN]
        xT = np.ascontiguousarray(xl.reshape(8, 512, 32, 128).transpose(0, 3, 2, 1)).reshape(8, 128, 32 * 512)
        m = dict(sh)
        m["xT"] = xT
        m["xown"] = np.ascontiguousarray(x[b, half * OWN:(half + 1) * OWN])
        m["pos"] = pl
        m.update(_consts(half))
        in_maps.append(m)
    nc = build_nc()
    res = run_bass_kernel_spmd(nc, in_maps, core_ids=list(range(8)))
    outp = np.zeros((4, 4096, D), np.float32)
    for c in range(8):
        b, half = c // 2, c % 2
        outp[b, half * OWN:(half + 1) * OWN] = np.asarray(res.results[c]["out"], np.float32)
    return outp
```
